# Optimizing a Trainium2 kernel written in Bass

```python
import jax, jax.numpy as jnp
from jax import lax
import numpy as np

D_MODEL = 2048
BATCH = 8
SEQ = 2048
DEPTH = 1

D_MIX = D_MODEL
D_RWKV = D_MIX // 2
D_FNET = D_MIX - D_RWKV
HEAD_DIM = 64
N_HEADS_RWKV = D_RWKV // HEAD_DIM
FNET_GROUP = 64
N_FNET_GROUPS = D_FNET // FNET_GROUP
N_DIR = 2
DECAY_LORA = 64
ICLR_LORA = 64
GATE_LORA = 160
D_SHIFT = 3 * D_RWKV + N_DIR * DECAY_LORA + N_DIR * ICLR_LORA + GATE_LORA
D_IN = D_SHIFT + D_FNET
N_EXPERTS = 32
TOP_K = 4
D_EXPERT = D_MODEL
SWIGLU_LIMIT = 7.0
SWIGLU_ALPHA = 1.702
ROUTE_BLOCK = 128
DEEPNORM_ALPHA = (2.0 * DEPTH) ** 0.25
DEEPNORM_BETA = (8.0 * DEPTH) ** -0.25
LN_EPS = 1e-5
GN_EPS = 64e-5

kernel_name = "rwkv7_fnet_hymba_moe_deepnorm"


def layer_norm(x, g, b):
    xf = x.astype(jnp.float32)
    mu = xf.mean(-1, keepdims=True)
    var = jnp.square(xf - mu).mean(-1, keepdims=True)
    return ((xf - mu) * lax.rsqrt(var + LN_EPS) * g.astype(jnp.float32) + b.astype(jnp.float32)).astype(x.dtype)


def centred_shift(t):
    p = jnp.pad(t, ((0, 0), (1, 1), (0, 0)))
    return 0.5 * (p[:, :-2] + p[:, 2:])


def stack_dirs(t):
    return jnp.stack([t, jnp.flip(t, axis=1)])


def flip_second(t):
    return jnp.stack([t[0], jnp.flip(t[1], axis=1)])


def wkv7_scan(r, w, k, v, kk, a):
    xs = tuple(jnp.moveaxis(t.astype(jnp.float32), 2, 0) for t in (r, w, k, v, kk, a))

    def step(S, inp):
        r_t, w_t, k_t, v_t, kk_t, a_t = inp
        sa = jnp.einsum('dbhvk,dbhk->dbhv', S, -kk_t)
        S = (S * w_t[..., None, :]
             + sa[..., None] * (kk_t * a_t)[..., None, :]
             + v_t[..., None] * k_t[..., None, :])
        y = jnp.einsum('dbhvk,dbhk->dbhv', S, r_t)
        return S, y

    d, b, _, h, n = r.shape
    S0 = jnp.zeros((d, b, h, n, n), jnp.float32)
    _, ys = lax.scan(step, S0, xs)
    return jnp.moveaxis(ys, 0, 2)


def mixer(x, w_in, mu_shift, w0, w2, a0, a2, g2, k_k, k_a, r_k, lnx_g, lnx_b, beta_f, w_out):
    B, T, _ = x.shape
    heads = lambda t: t.reshape(t.shape[:-1] + (N_HEADS_RWKV, HEAD_DIM))
    proj = x @ w_in
    ps = proj[..., :D_SHIFT]
    ps = ps + mu_shift * (centred_shift(ps) - ps)
    f = proj[..., D_SHIFT:]
    c1 = 3 * D_RWKV + N_DIR * DECAY_LORA
    r, k, v, wd, ad, gd = jnp.split(
        ps, [D_RWKV, 2 * D_RWKV, 3 * D_RWKV, c1, c1 + N_DIR * ICLR_LORA], axis=-1)
    wd = wd.reshape(B, T, N_DIR, DECAY_LORA)
    ad = ad.reshape(B, T, N_DIR, ICLR_LORA)
    wl = (jnp.einsum('btdr,drc->dbtc', jnp.tanh(wd), w2) + w0[:, None, None, :]).astype(jnp.float32)
    decay = jnp.exp(-jnp.exp(-jax.nn.softplus(-wl) - 0.5))
    a = jax.nn.sigmoid((jnp.einsum('btdr,drc->dbtc', ad, a2) + a0[:, None, None, :]).astype(jnp.float32))
    g = (jax.nn.sigmoid(gd) @ g2).astype(jnp.float32)
    r, k, v = heads(r).astype(jnp.float32), heads(k).astype(jnp.float32), heads(v).astype(jnp.float32)
    decay, a = heads(decay), heads(a)
    kk = k * heads(k_k).astype(jnp.float32)
    kk = kk / jnp.maximum(jnp.sqrt(jnp.sum(kk * kk, -1, keepdims=True)), 1e-12)
    k_dir = k[None] * (1.0 + (a - 1.0) * heads(k_a).astype(jnp.float32))
    ys = wkv7_scan(stack_dirs(r), flip_second(decay), flip_second(k_dir),
                   stack_dirs(v), stack_dirs(kk), flip_second(a))
    y = ys[0] + jnp.flip(ys[1], axis=1)
    mu = y.mean(-1, keepdims=True)
    var = jnp.square(y - mu).mean(-1, keepdims=True)
    y = ((y - mu) * lax.rsqrt(var + GN_EPS)).reshape(B, T, D_RWKV)
    y = y * lnx_g.astype(jnp.float32) + lnx_b.astype(jnp.float32)
    bonus = (jnp.sum(r * k * r_k.astype(jnp.float32), -1, keepdims=True) * v).reshape(B, T, D_RWKV)
    out_rwkv = (y + bonus) * g
    fg = f.reshape(B, T, N_FNET_GROUPS, FNET_GROUP).astype(jnp.float32)
    ff = jnp.fft.fftn(fg, axes=(1, 3), norm='ortho').real
    out_f = ff.reshape(B, T, D_FNET) * beta_f.astype(jnp.float32)
    mix = jnp.concatenate([out_rwkv.astype(x.dtype), out_f.astype(x.dtype)], axis=-1)
    return mix @ w_out


def moe(x, w_router, b_router, w_gu, b_gu, w_down, b_down):
    B, T, D = x.shape
    N = B * T
    M = N * TOP_K
    h = x.reshape(N, D)
    logits = (h @ w_router + b_router).astype(jnp.float32)
    top_val, top_idx = lax.top_k(logits, TOP_K)
    gates = jax.nn.softmax(top_val, axis=-1)
    n_blocks = -(-M // ROUTE_BLOCK) + N_EXPERTS
    n_rows = n_blocks * ROUTE_BLOCK
    flat_e = top_idx.reshape(M).astype(jnp.int32)
    order = jnp.argsort(flat_e, stable=True).astype(jnp.int32)
    sorted_e = flat_e[order]
    counts = jnp.bincount(flat_e, length=N_EXPERTS).astype(jnp.int32)
    starts = jnp.cumsum(counts) - counts
    padded = (counts + ROUTE_BLOCK - 1) // ROUTE_BLOCK * ROUTE_BLOCK
    pends = jnp.cumsum(padded)
    pstarts = pends - padded
    dest_sorted = pstarts[sorted_e] + jnp.arange(M, dtype=jnp.int32) - starts[sorted_e]
    row_token = jnp.full((n_rows,), N, jnp.int32).at[dest_sorted].set(order // TOP_K)
    block_expert = jnp.minimum(
        jnp.searchsorted(pends, jnp.arange(n_blocks, dtype=jnp.int32) * ROUTE_BLOCK, side='right'),
        N_EXPERTS - 1).astype(jnp.int32)
    h_pad = jnp.concatenate([h, jnp.zeros((1, D), h.dtype)], axis=0)

    def expert_block(args):
        rows, e = args
        xb = h_pad[rows]
        gu = xb @ w_gu[e] + b_gu[e]
        glu = jnp.minimum(gu[:, :D_EXPERT], SWIGLU_LIMIT)
        lin = jnp.clip(gu[:, D_EXPERT:], -SWIGLU_LIMIT, SWIGLU_LIMIT)
        act = glu * jax.nn.sigmoid(SWIGLU_ALPHA * glu) * (lin + 1.0)
        return act @ w_down[e] + b_down[e]

    out_rows = lax.map(expert_block, (row_token.reshape(n_blocks, ROUTE_BLOCK), block_expert))
    out_rows = out_rows.reshape(n_rows, D)
    dest = jnp.zeros((M,), jnp.int32).at[order].set(dest_sorted)
    y = jnp.einsum('nkd,nk->nd', out_rows[dest].reshape(N, TOP_K, D), gates.astype(out_rows.dtype))
    return y.reshape(B, T, D)


def setup_inputs(seed: int = 0) -> dict:
    key = jax.random.key(seed)
    ks = jax.random.split(key, 26)
    L = DEPTH
    nrm = lambda k, s, sc: jax.random.normal(k, s, jnp.float32) * sc
    return {
        "x": jax.random.normal(ks[0], (BATCH, SEQ, D_MODEL), jnp.float32),
        "w_in": nrm(ks[1], (L, D_MODEL, D_IN), D_MODEL ** -0.5),
        "mu_shift": jax.random.uniform(ks[2], (L, D_SHIFT), jnp.float32),
        "w0": jax.random.uniform(ks[3], (L, N_DIR, D_RWKV), jnp.float32, -6.0, -1.0),
        "w2": nrm(ks[4], (L, N_DIR, DECAY_LORA, D_RWKV), 0.1 * DECAY_LORA ** -0.5),
        "a0": nrm(ks[5], (L, N_DIR, D_RWKV), 0.5),
        "a2": nrm(ks[6], (L, N_DIR, ICLR_LORA, D_RWKV), ICLR_LORA ** -0.5),
        "g2": nrm(ks[7], (L, GATE_LORA, D_RWKV), GATE_LORA ** -0.5),
        "k_k": 0.85 + nrm(ks[8], (L, D_RWKV), 0.1),
        "k_a": 1.0 + nrm(ks[9], (L, D_RWKV), 0.1),
        "r_k": nrm(ks[10], (L, N_HEADS_RWKV, HEAD_DIM), 0.1),
        "lnx_g": 1.0 + nrm(ks[11], (L, D_RWKV), 0.1),
        "lnx_b": nrm(ks[12], (L, D_RWKV), 0.02),
        "beta_f": 1.0 + nrm(ks[13], (L, D_FNET), 0.1),
        "w_out": nrm(ks[14], (L, D_MIX, D_MODEL), DEEPNORM_BETA * D_MIX ** -0.5),
        "ln1_g": 1.0 + nrm(ks[15], (L, D_MODEL), 0.1),
        "ln1_b": nrm(ks[16], (L, D_MODEL), 0.02),
        "w_router": nrm(ks[17], (L, D_MODEL, N_EXPERTS), D_MODEL ** -0.5),
        "b_router": nrm(ks[18], (L, N_EXPERTS), 0.01),
        "w_gu": nrm(ks[19], (L, N_EXPERTS, D_MODEL, 2 * D_EXPERT), D_MODEL ** -0.5),
        "b_gu": nrm(ks[20], (L, N_EXPERTS, 2 * D_EXPERT), 0.02),
        "w_down": nrm(ks[21], (L, N_EXPERTS, D_EXPERT, D_MODEL), DEEPNORM_BETA * D_EXPERT ** -0.5),
        "b_down": nrm(ks[22], (L, N_EXPERTS, D_MODEL), 0.02),
        "ln2_g": 1.0 + nrm(ks[23], (L, D_MODEL), 0.1),
        "ln2_b": nrm(ks[24], (L, D_MODEL), 0.02),
    }


def reference(x, w_in, mu_shift, w0, w2, a0, a2, g2, k_k, k_a, r_k, lnx_g, lnx_b, beta_f, w_out,
              ln1_g, ln1_b, w_router, b_router, w_gu, b_gu, w_down, b_down, ln2_g, ln2_b):
    for l in range(DEPTH):
        m = mixer(x, w_in[l], mu_shift[l], w0[l], w2[l], a0[l], a2[l], g2[l], k_k[l], k_a[l], r_k[l],
                  lnx_g[l], lnx_b[l], beta_f[l], w_out[l])
        x = layer_norm(DEEPNORM_ALPHA * x + m, ln1_g[l], ln1_b[l])
        e = moe(x, w_router[l], b_router[l], w_gu[l], b_gu[l], w_down[l], b_down[l])
        x = layer_norm(DEEPNORM_ALPHA * x + e, ln2_g[l], ln2_b[l])
    return x
```

```python
import numpy as np
from contextlib import ExitStack
import concourse.bass as bass
import concourse.mybir as mybir
from concourse.bass_utils import run_bass_kernel_spmd

F32 = mybir.dt.float32
BF16 = mybir.dt.bfloat16
I32 = mybir.dt.int32
AF = mybir.ActivationFunctionType
ALU = mybir.AluOpType

T = 2048
D = 2048
NT = 16
NE = 32
CAP = 384
NJ = CAP // 128
KAPPA = -float(np.exp(-0.5))
ALPHA = float(2.0 ** 0.25)
LN_EPS = 1e-5
GN_EPS = 64e-5
D_SHIFT = 3488
D_IN = 4512
ENG = ("pe", "act", "dve", "pool", "sp")
NDMA = 8


class Buf:
    __slots__ = ("w", "r", "const", "excl")

    def __init__(self, const=False):
        self.w = {}
        self.r = {}
        self.const = const
        self.excl = False


class Tl:
    def __init__(self, t, const=False):
        self.t = t
        self.b = Buf(const)


class Sched:
    def __init__(self, nc, es):
        self.nc = nc
        self.e = {"pe": nc.tensor, "act": nc.scalar, "dve": nc.vector, "pool": nc.gpsimd, "sp": nc.sync}
        self.sem = {k: es.enter_context(nc.semaphore("s_" + k)) for k in ENG}
        self.cnt = {k: 0 for k in ENG}
        self.pend = {k: False for k in ENG}
        self.dsem = {q: [es.enter_context(nc.semaphore("d_%s%d" % (q, i))) for i in range(NDMA)]
                     for q in ("sp", "act", "pool")}
        self.dcnt = {q: 0 for q in self.dsem}
        self.waited = {k: {} for k in ENG}

    def _wait(self, eng, evs):
        w = self.waited[eng]
        for sem, val in evs:
            if w.get(sem, 0) >= val:
                continue
            self.e[eng].wait_ge(sem, val)
            w[sem] = val

    def _deps(self, eng, reads, writes, is_dma):
        own = self.sem[eng]
        evs = []
        for b in reads:
            for sem, val in b.w.items():
                if sem is own and not is_dma and eng == "pe":
                    continue
                evs.append((sem, val))
        for b in writes:
            for sem, val in b.w.items():
                if sem is own and not is_dma:
                    continue
                evs.append((sem, val))
            for sem, val in b.r.items():
                if sem is own and not is_dma:
                    continue
                evs.append((sem, val))
        return evs

    def _record(self, ev, reads, writes, merge=False):
        sem, val = ev
        for b in reads:
            if not b.const:
                b.r[sem] = max(b.r.get(sem, 0), val)
        for b in writes:
            if merge:
                b.w = dict(b.w)
                b.w[sem] = max(b.w.get(sem, 0), val)
            else:
                b.w = {sem: val}
            b.r = {}

    def op(self, eng, fn, reads=(), writes=(), inc=True):
        if any(b.excl for b in reads):
            writes = list(writes) + [b for b in reads if b.excl]
            reads = [b for b in reads if not b.excl]
        self._wait(eng, self._deps(eng, reads, writes, False))
        ins = fn(self.e[eng])
        if inc:
            self.cnt[eng] += 1
            ins.then_inc(self.sem[eng], 1)
            ev = (self.sem[eng], self.cnt[eng])
            self.pend[eng] = False
        else:
            ev = (self.sem[eng], self.cnt[eng] + 1)
            self.pend[eng] = True
        self._record(ev, reads, writes)

    def _dma_ev(self, q):
        k = self.dcnt[q]
        self.dcnt[q] += 1
        sem = self.dsem[q][k % NDMA]
        pre = [(sem, 16 * (k // NDMA))] if k >= NDMA else []
        return sem, 16 * (k // NDMA + 1), pre

    def dma(self, q, out, in_, reads=(), writes=()):
        sem, tgt, pre = self._dma_ev(q)
        self._wait(q, self._deps(q, reads, writes, True) + pre)
        self.e[q].dma_start(out=out, in_=in_).then_inc(sem, 16)
        self._record((sem, tgt), reads, writes, merge=True)

    def gather(self, out, table, idx_ap, reads=(), writes=()):
        q = "pool"
        sem, tgt, pre = self._dma_ev(q)
        self._wait(q, self._deps(q, reads, writes, True) + pre)
        self.e[q].indirect_dma_start(out=out, out_offset=None, in_=table,
                                     in_offset=bass.IndirectOffsetOnAxis(ap=idx_ap, axis=0)).then_inc(sem, 16)
        self._record((sem, tgt), reads, writes)

    def barrier(self):
        evs = []
        for k in ENG:
            assert not self.pend[k]
            if self.cnt[k] > 0:
                evs.append((self.sem[k], self.cnt[k]))
        for q, sems in self.dsem.items():
            n = self.dcnt[q]
            for i, sem in enumerate(sems):
                ni = (n - i + NDMA - 1) // NDMA if n > i else 0
                if ni > 0:
                    evs.append((sem, 16 * ni))
        for k in ENG:
            self._wait(k, [ev for ev in evs if ev[0] is not self.sem[k]])


def _interleave(gens):
    gens = list(gens)
    while gens:
        for g in list(gens):
            try:
                next(g)
            except StopIteration:
                gens.remove(g)


def _cst_layout():
    names = [("TRI0", 256), ("TRI1", 256), ("IDF", 128), ("BO64", 128), ("BO1", 128), ("IOTA", 384), ("EBASE", 32),
             ("TRIS", 128), ("CSD", 256), ("MXX", 1024), ("M30", 768), ("M31", 768)]
    off = {}
    o = 0
    for n, w in names:
        off[n] = (o, o + w)
        o += w
    return off, o


CST_OFF, CST_N = _cst_layout()


def _consts():
    c = np.zeros((128, CST_N), np.float32)
    p = np.arange(128)[:, None]
    q = np.arange(128)[None, :]

    def put(name, arr):
        a, b = CST_OFF[name]
        c[:, a:b] = arr

    put("TRI0", np.concatenate([(p <= q) * KAPPA, (p < q) * KAPPA], 1))
    put("TRI1", np.concatenate([(p >= q) * KAPPA, (p > q) * KAPPA], 1))
    put("IDF", (p == q) * 1.0)
    put("BO64", ((p // 64) == (q // 64)) / 64.0)
    put("BO1", ((p // 64) == (q // 64)) * 1.0)
    put("IOTA", np.broadcast_to(np.arange(384)[None, :], (128, 384)))
    put("EBASE", np.broadcast_to((np.arange(32) * CAP)[None, :], (128, 32)))
    put("TRIS", (p < q) * 1.0)
    cc = np.cos(2 * np.pi * (p % 64) * (q % 64) / 64.0) * ((p // 64) == (q // 64))
    ss = np.sin(2 * np.pi * (p % 64) * (q % 64) / 64.0) * ((p // 64) == (q // 64))
    put("CSD", np.concatenate([cc, ss], 1))
    xt0, x0 = (q > p) * 1.0, (q < p) * 1.0
    xt1, x1 = (q < p) * 1.0, (q > p) * 1.0
    put("MXX", np.concatenate([xt0, x0, xt0, x0, xt1, x1, xt1, x1], 1))
    in0, st0 = (q >= p) * 1.0, (q > p) * 1.0
    in1, st1 = (q <= p) * 1.0, (q < p) * 1.0
    put("M30", np.concatenate([in0, st0, in0, in0, st0, in0], 1))
    put("M31", np.concatenate([in1, st1, in1, in1, st1, in1], 1))
    return c


def build(debug=None):
    nc = bass.Bass("TRN2", target_bir_lowering=False)
    debug = debug or {}
    upto = debug.get("upto", "Z")
    ORDER = "AFBSOECZ"
    at_least = lambda st: ORDER.index(upto) >= ORDER.index(st)

    def din(name, shape, dt=F32):
        return nc.dram_tensor(name, list(shape), dt, kind="ExternalInput").ap()

    def dscr(name, shape, dt=F32):
        kind = "ExternalOutput" if name in debug.get("dump", ()) else "Internal"
        return nc.dram_tensor(name, list(shape), dt, kind=kind).ap()

    xT = din("xT", [D, T])
    x_tm = din("x_tm", [T, D])
    w_in = din("w_in", [D, D_IN])
    mu_t = din("mu_t", [128, 28])
    w2e = din("w2e", [2, 65, 1024])
    a2 = din("a2", [2, 64, 1024])
    g2 = din("g2", [160, 1024])
    vecs = din("vecs", [128, 72])
    w_out = din("w_out", [D, D])
    rows = din("rows", [6, D])
    w_router = din("w_router", [D, NE])
    w_gu = din("w_gu", [NE, D, 2 * D]) if at_least("E") else None
    bgu_t = din("bgu_t", [128, NE * 32])
    w_down = din("w_down", [NE, D, D]) if at_least("E") else None
    b_down = din("b_down", [NE, D])
    ctab = din("ctab", [T, T])
    stab = din("stab", [T, T])
    cst = din("cst", [128, CST_N])
    tid = din("tid", [128, 2 * NT])
    out = nc.dram_tensor("out", [T, D], F32, kind="ExternalOutput").ap()

    ps_scr = dscr("ps_scr", [3584, T])
    sg_scr = dscr("sg_scr", [2, T, 1024])
    a_scr = dscr("a_scr", [2, 1024, T])
    mix_scr = dscr("mix_scr", [D, T], BF16)
    h1_scr = dscr("h1_scr", [T, D])
    h1b_scr = dscr("h1b_scr", [T, D], BF16)
    y_scr = dscr("y_scr", [NE * CAP, D])
    dbg = dscr("dbg", [T, D])

    es = ExitStack()
    with es:
        S = Sched(nc, es)

        def sb(stack, name, shape, dt=F32, const=False):
            return Tl(stack.enter_context(nc.sbuf_tensor(name, list(shape), dt)), const)

        pb = [Tl(es.enter_context(nc.psum_tensor("pb%d" % i, [128, 512], F32))) for i in range(7)]
        ptr = Tl(es.enter_context(nc.psum_tensor("ptr", [128, 1024], BF16)))
        for tl in pb + [ptr]:
            tl.b.excl = True

        CPS = sb(es, "cst2_sb", [128, 800], F32)
        S.dma("sp", CPS.t[:], cst[:, 512:1312], writes=[CPS.b])
        POSM = sb(es, "posm", [128, NT, NE], F32)
        DEST = sb(es, "dest", [128, NT * 4], I32)
        GK = sb(es, "gk", [128, NT * 4], F32)
        CB = sb(es, "cst_bf", [128, 1280], BF16)
        VEC = sb(es, "vecs_sb", [128, 72], F32)
        VX = sb(es, "vecx_sb", [128, 16], F32)
        sC = ExitStack()
        C = sb(sC, "cst_sb", [128, CST_N], F32)
        S.dma("sp", C.t[:], cst, writes=[C.b])
        cs = lambda n: C.t[:, CST_OFF[n][0]:CST_OFF[n][1]]
        TRI = [cs("TRI0"), cs("TRI1")]
        IDF, BO64, BO1, IOTA, EBASE = cs("IDF"), cs("BO64"), cs("BO1"), cs("IOTA"), cs("EBASE")
        MXX, M3 = cs("MXX"), [cs("M30"), cs("M31")]
        S.op("dve", lambda e: e.tensor_copy(out=CB.t[:, 0:128], in_=IDF), reads=[C.b], writes=[CB.b])
        for i in range(4):
            S.op("dve", lambda e: e.tensor_copy(out=CB.t[:, 128 + 128 * i:256 + 128 * i], in_=IDF), reads=[C.b],
                 writes=[CB.b])
        S.op("dve", lambda e: e.tensor_copy(out=CB.t[:, 640:768], in_=cs("TRIS")), reads=[C.b], writes=[CB.b])
        S.op("dve", lambda e: e.memset(CB.t[:, 768:1024], 1.0), writes=[CB.b])
        S.op("dve", lambda e: e.tensor_copy(out=CB.t[:, 1024:1280], in_=cs("CSD")), reads=[C.b], writes=[CB.b])
        IDB, ID4, TRISB, ONESB, CSDB = CB.t[:, 0:128], CB.t[:, 128:640], CB.t[:, 640:768], CB.t[:, 768:896], CB.t[:, 1024:1280]
        S.dma("sp", VEC.t[:], vecs, writes=[VEC.b])
        S.op("dve", lambda e: e.tensor_scalar(out=VX.t[:, 0:8], in0=VEC.t[:, 24:32], scalar1=-1.0, scalar2=1.0,
                                              op0=ALU.mult, op1=ALU.add), reads=[VEC.b], writes=[VX.b])
        S.op("dve", lambda e: e.tensor_scalar(out=VX.t[:, 8:16], in0=VEC.t[:, 56:64],
                                              scalar1=float(1.0 / np.sqrt(T * 64.0)), scalar2=None, op0=ALU.mult),
             reads=[VEC.b], writes=[VX.b])
        S.barrier()
        for tl in (C, CPS, CB, VEC, VX):
            tl.b.const = True
        A0 = lambda d, cb: VEC.t[:, d * 8 + cb:d * 8 + cb + 1]
        KK_ = lambda cb: VEC.t[:, 16 + cb:17 + cb]
        KA_ = lambda cb: VEC.t[:, 24 + cb:25 + cb]
        RK_ = lambda cb: VEC.t[:, 32 + cb:33 + cb]
        LG_ = lambda cb: VEC.t[:, 40 + cb:41 + cb]
        LB_ = lambda cb: VEC.t[:, 48 + cb:49 + cb]
        OMKA = lambda cb: VX.t[:, cb:cb + 1]
        BFS = lambda cb: VX.t[:, 8 + cb:9 + cb]

        def finish():
            S.barrier()

        sF = ExitStack()
        fT = sb(sF, "fT", [128, 8, T], BF16)
        with ExitStack() as sA:
            xTb = sb(sA, "xTb", [128, 16, T], BF16)
            for kc in range(16):
                S.dma("pool", xTb.t[:, kc, :], xT[kc * 128:(kc + 1) * 128, :], writes=[xTb.b])
            MU = sb(sA, "mu_sb", [128, 28], F32)
            C1 = sb(sA, "c1_sb", [128, 28], F32)
            C2 = sb(sA, "c2_sb", [128, 28], F32)
            S.dma("sp", MU.t[:], mu_t, writes=[MU.b])
            S.op("dve", lambda e: e.tensor_scalar(out=C1.t[:], in0=MU.t[:], scalar1=-1.0, scalar2=1.0, op0=ALU.mult,
                                                  op1=ALU.add), reads=[MU.b], writes=[C1.b])
            S.op("dve", lambda e: e.tensor_scalar(out=C2.t[:], in0=MU.t[:], scalar1=0.5, scalar2=None, op0=ALU.mult),
                 reads=[MU.b], writes=[C2.b])
            wring = [sb(sA, "wblk%d" % i, [128, 16, 128], BF16) for i in range(2)]
            Pr = [sb(sA, "Pr%d" % i, [128, T], F32) for i in range(2)]
            Sx = sb(sA, "Sx", [128, T], F32)
            Or = [sb(sA, "Or%d" % i, [128, T], F32) for i in range(2)]
            blocks = [(c0, 128) for c0 in range(0, 3328, 128)] + [(3328, 128), (3456, 32)] + \
                     [(D_SHIFT + 128 * i, 128) for i in range(8)]
            w_in_v = w_in.rearrange("(kc p) c -> p kc c", p=128)
            nbank = 0
            for bi, (c0, n) in enumerate(blocks):
                wb = wring[bi % 2]
                S.dma("pool", wb.t[:, :, 0:n], w_in_v[:, :, c0:c0 + n], writes=[wb.b])
                isf = bi >= 28
                P = Pr[bi % 2]
                for tq in range(4):
                    bank = pb[nbank % 4]
                    nbank += 1
                    for kc in range(16):
                        S.op("pe", lambda e: e.matmul(bank.t[0:n, :], lhsT=wb.t[:, kc, 0:n],
                                                      rhs=xTb.t[:, kc, tq * 512:(tq + 1) * 512],
                                                      start=(kc == 0), stop=(kc == 15)),
                             reads=[wb.b, xTb.b], writes=[bank.b], inc=(kc == 15))
                    if isf:
                        S.op("act", lambda e: e.activation(out=fT.t[:, bi - 28, tq * 512:(tq + 1) * 512],
                                                           in_=bank.t[:, :], func=AF.Copy),
                             reads=[bank.b], writes=[fT.b])
                    else:
                        S.op("act", lambda e: e.activation(out=P.t[0:n, tq * 512:(tq + 1) * 512], in_=bank.t[0:n, :],
                                                           func=AF.Copy), reads=[bank.b], writes=[P.b])
                if isf:
                    continue
                O = Or[bi % 2]
                S.op("dve", lambda e: e.tensor_tensor(out=Sx.t[0:n, 1:T - 1], in0=P.t[0:n, 0:T - 2], in1=P.t[0:n, 2:T],
                                                      op=ALU.add), reads=[P.b], writes=[Sx.b])
                S.op("dve", lambda e: e.tensor_copy(out=Sx.t[0:n, 0:1], in_=P.t[0:n, 1:2]), reads=[P.b], writes=[Sx.b])
                S.op("dve", lambda e: e.tensor_copy(out=Sx.t[0:n, T - 1:T], in_=P.t[0:n, T - 2:T - 1]), reads=[P.b],
                     writes=[Sx.b])
                S.op("dve", lambda e: e.tensor_scalar(out=Sx.t[0:n, :], in0=Sx.t[0:n, :], scalar1=C2.t[0:n, bi:bi + 1],
                                                      scalar2=None, op0=ALU.mult), reads=[Sx.b, C2.b], writes=[Sx.b])
                S.op("dve", lambda e: e.scalar_tensor_tensor(out=O.t[0:n, :], in0=P.t[0:n, :], scalar=C1.t[0:n, bi:bi + 1],
                                                             in1=Sx.t[0:n, :], op0=ALU.mult, op1=ALU.add),
                     reads=[P.b, Sx.b, C1.b], writes=[O.b])
                S.dma("sp", ps_scr[c0:c0 + n, :], O.t[0:n, :], reads=[O.b])
            S.barrier()
        if upto == "A":
            sF.close()
            sC.close()
            return nc

        with sF:
            Fc = sb(sF, "Fc", [128, NT, 1024], BF16)
            Fs = sb(sF, "Fs", [128, NT, 1024], BF16)
            for tt in range(NT):
                bc = [pb[0], pb[1]]
                bs_ = [pb[2], pb[3]]
                for blk in range(8):
                    S.op("pe", lambda e: e.matmul(bc[blk // 4].t[:, (blk % 4) * 128:(blk % 4 + 1) * 128],
                                                  lhsT=fT.t[:, blk, tt * 128:(tt + 1) * 128], rhs=CSDB[:, 0:128],
                                                  start=True, stop=True), reads=[fT.b, CB.b], writes=[bc[blk // 4].b],
                         inc=False)
                    S.op("pe", lambda e: e.matmul(bs_[blk // 4].t[:, (blk % 4) * 128:(blk % 4 + 1) * 128],
                                                  lhsT=fT.t[:, blk, tt * 128:(tt + 1) * 128], rhs=CSDB[:, 128:256],
                                                  start=True, stop=True), reads=[fT.b, CB.b], writes=[bs_[blk // 4].b],
                         inc=(blk == 7))
                for h in range(2):
                    S.op("act", lambda e: e.activation(out=Fc.t[:, tt, h * 512:(h + 1) * 512], in_=bc[h].t[:, :],
                                                       func=AF.Copy), reads=[bc[h].b], writes=[Fc.b])
                    S.op("dve", lambda e: e.tensor_copy(out=Fs.t[:, tt, h * 512:(h + 1) * 512], in_=bs_[h].t[:, :]),
                         reads=[bs_[h].b], writes=[Fs.b])
            ctv = ctab.rearrange("(tt p) q -> p tt q", p=128)
            stv = stab.rearrange("(tt p) q -> p tt q", p=128)
            CTr = [sb(sF, "CT%d" % i, [128, NT, 512], BF16) for i in range(2)]
            STr = [sb(sF, "ST%d" % i, [128, NT, 512], BF16) for i in range(2)]
            Mo = [sb(sF, "Mo%d" % i, [128, 512], BF16) for i in range(2)]
            nb = 0
            for tq in range(4):
                CT, ST = CTr[tq % 2], STr[tq % 2]
                S.dma("pool", CT.t[:], ctv[:, :, tq * 512:(tq + 1) * 512], writes=[CT.b])
                S.dma("pool", ST.t[:], stv[:, :, tq * 512:(tq + 1) * 512], writes=[ST.b])
                for blk in range(8):
                    bank = pb[nb % 4]
                    mo = Mo[nb % 2]
                    nb += 1
                    for tt in range(NT):
                        S.op("pe", lambda e: e.matmul(bank.t[:, :], lhsT=Fc.t[:, tt, blk * 128:(blk + 1) * 128],
                                                      rhs=CT.t[:, tt, :], start=(tt == 0), stop=False),
                             reads=[Fc.b, CT.b], writes=[bank.b], inc=False)
                        S.op("pe", lambda e: e.matmul(bank.t[:, :], lhsT=Fs.t[:, tt, blk * 128:(blk + 1) * 128],
                                                      rhs=ST.t[:, tt, :], start=False, stop=(tt == NT - 1)),
                             reads=[Fs.b, ST.b], writes=[bank.b], inc=(tt == NT - 1))
                    S.op("act", lambda e: e.activation(out=mo.t[:, :], in_=bank.t[:, :], func=AF.Identity,
                                                       scale=BFS(blk)), reads=[bank.b], writes=[mo.b])
                    S.dma("sp", mix_scr[1024 + blk * 128:1024 + (blk + 1) * 128, tq * 512:(tq + 1) * 512], mo.t[:, :],
                          reads=[mo.b])
            S.barrier()
        if upto == "F":
            sC.close()
            return nc

        sG = ExitStack()
        SGD0 = sb(sG, "sgd0", [128, T], BF16)
        SGD1 = sb(sG, "sgd1", [32, T], BF16)
        G2A = sb(sG, "g2a", [128, 1024], BF16)
        G2B = sb(sG, "g2b", [32, 1024], BF16)
        S.dma("pool", G2A.t[:], g2[0:128, :], writes=[G2A.b])
        S.dma("pool", G2B.t[:], g2[128:160, :], writes=[G2B.b])
        with ExitStack() as sB:
            stg = sb(sB, "stg", [128, T], F32)
            S.dma("sp", stg.t[:], ps_scr[3328:3456, :], writes=[stg.b])
            S.op("act", lambda e: e.activation(out=SGD0.t[:], in_=stg.t[:], func=AF.Sigmoid), reads=[stg.b], writes=[SGD0.b])
            stg2 = sb(sB, "stg2", [32, T], F32)
            S.dma("sp", stg2.t[:], ps_scr[3456:3488, :], writes=[stg2.b])
            S.op("act", lambda e: e.activation(out=SGD1.t[:], in_=stg2.t[:], func=AF.Sigmoid), reads=[stg2.b], writes=[SGD1.b])
            nb = 0
            for d in range(2):
                wdt = sb(sB, "wdt%d" % d, [65, T], F32)
                w2s = sb(sB, "w2s%d" % d, [65, 1024], F32)
                S.dma("sp", wdt.t[0:64, :], ps_scr[3072 + 64 * d:3136 + 64 * d, :], writes=[wdt.b])
                S.dma("sp", w2s.t[:], w2e[d], writes=[w2s.b])
                S.op("act", lambda e: e.activation(out=wdt.t[0:64, :], in_=wdt.t[0:64, :], func=AF.Tanh), reads=[wdt.b],
                     writes=[wdt.b])
                S.op("dve", lambda e: e.memset(wdt.t[64:65, :], 1.0), writes=[wdt.b])
                SGt = [sb(sB, "sgt%d_%d" % (d, i), [128, 1024], F32) for i in range(2)]
                for tt in range(NT):
                    sg = SGt[tt % 2]
                    for h in range(2):
                        bank = pb[nb % 4]
                        nb += 1
                        S.op("pe", lambda e: e.matmul(bank.t[:, :], lhsT=wdt.t[:, tt * 128:(tt + 1) * 128],
                                                      rhs=w2s.t[:, h * 512:(h + 1) * 512], start=True, stop=True),
                             reads=[wdt.b, w2s.b], writes=[bank.b])
                        S.op("act", lambda e: e.activation(out=sg.t[:, h * 512:(h + 1) * 512], in_=bank.t[:, :],
                                                           func=AF.Sigmoid), reads=[bank.b], writes=[sg.b])
                    S.dma("sp", sg_scr[d, tt * 128:(tt + 1) * 128, :], sg.t[:], reads=[sg.b])
                adt = sb(sB, "adt%d" % d, [64, T], F32)
                a2s = sb(sB, "a2s%d" % d, [64, 1024], F32)
                S.dma("sp", adt.t[:], ps_scr[3200 + 64 * d:3264 + 64 * d, :], writes=[adt.b])
                S.dma("sp", a2s.t[:], a2[d], writes=[a2s.b])
                At = [sb(sB, "at%d_%d" % (d, i), [128, T], F32) for i in range(2)]
                for cb in range(8):
                    at = At[cb % 2]
                    for tq in range(4):
                        bank = pb[nb % 4]
                        nb += 1
                        S.op("pe", lambda e: e.matmul(bank.t[:, :], lhsT=a2s.t[:, cb * 128:(cb + 1) * 128],
                                                      rhs=adt.t[:, tq * 512:(tq + 1) * 512], start=True, stop=True),
                             reads=[adt.b, a2s.b], writes=[bank.b])
                        S.op("act", lambda e: e.activation(out=at.t[:, tq * 512:(tq + 1) * 512], in_=bank.t[:, :],
                                                           func=AF.Sigmoid, bias=A0(d, cb)), reads=[bank.b], writes=[at.b])
                    S.dma("sp", a_scr[d, cb * 128:(cb + 1) * 128, :], at.t[:], reads=[at.b])
            S.barrier()
        if upto == "B":
            sG.close()
            sC.close()
            return nc

        with sG, ExitStack() as sS:
            R_ = sb(sS, "r_sb", [128, T], F32)
            K_ = sb(sS, "k_sb", [128, T], F32)
            V_ = sb(sS, "v_sb", [128, T], F32)
            KKt = sb(sS, "kk_sb", [128, T], F32)
            RKt = sb(sS, "rk_sb", [128, T], F32)
            TMP = sb(sS, "tmp_sb", [128, T], F32)
            TMP2 = sb(sS, "tmp2_sb", [128, T], F32)
            Vb = sb(sS, "v_bf", [128, T], BF16)
            A_ = sb(sS, "a_sb", [128, T], F32)
            SGS = sb(sS, "sgslab", [128, NT, 128], F32)
            EG = sb(sS, "eg", [128, T], F32)
            EGX = sb(sS, "egx", [128, T], F32)
            ENG_ = sb(sS, "eng", [128, T], F32)
            GAM = [sb(sS, "gam%d" % d, [128, NT], F32) for d in range(2)]
            OPS = [sb(sS, "ops%d" % d, [128, 4, T], BF16) for d in range(2)]
            YD = [sb(sS, "yd%d" % d, [128, T], F32) for d in range(2)]
            TMr = [sb(sS, "tm%d" % i, [128, 6, 128], BF16) for i in range(2)]
            ATr = [sb(sS, "at3_%d" % i, [128, 4, 384], BF16) for i in range(2)]
            Qr = [sb(sS, "q%d" % i, [128, 4, 128], BF16) for i in range(2)]
            LVr = [sb(sS, "lv%d" % i, [128, 4, 256], BF16) for i in range(2)]
            Wsb = sb(sS, "wsb", [128, 256], BF16)
            Usb = sb(sS, "usb", [128, 256], BF16)
            Hf = sb(sS, "hf", [128, 2, 64], F32)
            Hb = sb(sS, "hb", [128, 2, 64], BF16)
            MIXo = sb(sS, "mixo", [128, T], BF16)
            PXXt = es.enter_context(nc.psum_tensor("pxx_dummy", [1, 1], F32)) if False else None
            PW = PU = pb[5].b
            PY = PH = pb[6].b
            pw_ap = lambda ii: pb[5].t[:, ii * 64:(ii + 1) * 64]
            pu_ap = lambda ii: pb[5].t[:, 256 + ii * 64:256 + (ii + 1) * 64]
            py_ap = lambda d, hh: pb[6].t[64 * hh:64 * hh + 64, d * 128:(d + 1) * 128]
            ph_ap = lambda d, hh: pb[6].t[64 * hh:64 * hh + 64, 256 + d * 64:256 + (d + 1) * 64]
            sgv = [sg_scr[d].rearrange("(tt j) c -> j tt c", j=128) for d in range(2)]

            def chunk_of(d, s):
                return s if d == 0 else NT - 1 - s

            for cb in range(8):
                S.dma("sp", R_.t[:], ps_scr[cb * 128:(cb + 1) * 128, :], writes=[R_.b])
                S.dma("sp", K_.t[:], ps_scr[1024 + cb * 128:1024 + (cb + 1) * 128, :], writes=[K_.b])
                S.dma("sp", V_.t[:], ps_scr[2048 + cb * 128:2048 + (cb + 1) * 128, :], writes=[V_.b])
                S.op("dve", lambda e: e.tensor_scalar(out=KKt.t[:], in0=K_.t[:], scalar1=KK_(cb), scalar2=None, op0=ALU.mult),
                     reads=[K_.b], writes=[KKt.b])
                S.op("dve", lambda e: e.tensor_tensor(out=TMP.t[:], in0=KKt.t[:], in1=KKt.t[:], op=ALU.mult), reads=[KKt.b],
                     writes=[TMP.b])
                for tq in range(4):
                    bank = pb[tq % 4]
                    S.op("pe", lambda e: e.matmul(bank.t[:, :], lhsT=BO1, rhs=TMP.t[:, tq * 512:(tq + 1) * 512], start=True,
                                                  stop=True), reads=[TMP.b], writes=[bank.b])
                    S.op("act", lambda e: e.activation(out=TMP2.t[:, tq * 512:(tq + 1) * 512], in_=bank.t[:, :], func=AF.Sqrt),
                         reads=[bank.b], writes=[TMP2.b])
                S.op("dve", lambda e: e.tensor_scalar(out=TMP2.t[:], in0=TMP2.t[:], scalar1=1e-12, scalar2=None, op0=ALU.max),
                     reads=[TMP2.b], writes=[TMP2.b])
                S.op("dve", lambda e: e.reciprocal(out=TMP2.t[:], in_=TMP2.t[:]), reads=[TMP2.b], writes=[TMP2.b])
                S.op("dve", lambda e: e.tensor_tensor(out=KKt.t[:], in0=KKt.t[:], in1=TMP2.t[:], op=ALU.mult),
                     reads=[KKt.b, TMP2.b], writes=[KKt.b])
                S.op("act", lambda e: e.activation(out=Vb.t[:], in_=V_.t[:], func=AF.Copy), reads=[V_.b], writes=[Vb.b])
                S.op("dve", lambda e: e.scalar_tensor_tensor(out=RKt.t[:], in0=R_.t[:], scalar=RK_(cb), in1=K_.t[:],
                                                             op0=ALU.mult, op1=ALU.mult), reads=[R_.b, K_.b], writes=[RKt.b])
                for d in range(2):
                    S.dma("sp", A_.t[:], a_scr[d, cb * 128:(cb + 1) * 128, :], writes=[A_.b])
                    S.dma("sp", SGS.t[:], sgv[d][:, :, cb * 128:(cb + 1) * 128], writes=[SGS.b])
                    for tp in range(8):
                        bank = pb[tp % 4]
                        for u in range(2):
                            tt = 2 * tp + u
                            S.op("pe", lambda e: e.matmul(bank.t[:, u * 256:(u + 1) * 256], lhsT=SGS.t[:, tt, :], rhs=TRI[d],
                                                          start=True, stop=True), reads=[SGS.b], writes=[bank.b], inc=(u == 1))
                        bv = bank.t[:, :].rearrange("p (u x i) -> p u x i", u=2, x=2)
                        dst = lambda X: X.t[:, tp * 256:(tp + 1) * 256].rearrange("p (u i) -> p u i", u=2)
                        S.op("act", lambda e: e.activation(out=dst(EG), in_=bv[:, :, 0, :], func=AF.Exp), reads=[bank.b],
                             writes=[EG.b])
                        S.op("act", lambda e: e.activation(out=dst(EGX), in_=bv[:, :, 1, :], func=AF.Exp), reads=[bank.b],
                             writes=[EGX.b])
                        S.op("act", lambda e: e.activation(out=dst(ENG_), in_=bv[:, :, 0, :], func=AF.Exp, scale=-1.0),
                             reads=[bank.b], writes=[ENG_.b])
                    gi = 127 if d == 0 else 0
                    S.op("dve", lambda e: e.tensor_copy(out=GAM[d].t[:],
                                                        in_=EG.t[:].rearrange("p (tt i) -> p tt i", i=128)[:, :, gi]),
                         reads=[EG.b], writes=[GAM[d].b])
                    S.op("dve", lambda e: e.scalar_tensor_tensor(out=OPS[d].t[:, 0, :], in0=KKt.t[:], scalar=-1.0, in1=EGX.t[:],
                                                                 op0=ALU.mult, op1=ALU.mult), reads=[KKt.b, EGX.b],
                         writes=[OPS[d].b])
                    S.op("dve", lambda e: e.tensor_tensor(out=OPS[d].t[:, 1, :], in0=R_.t[:], in1=EG.t[:], op=ALU.mult),
                         reads=[R_.b, EG.b], writes=[OPS[d].b])
                    S.op("dve", lambda e: e.tensor_tensor(out=TMP.t[:], in0=KKt.t[:], in1=A_.t[:], op=ALU.mult),
                         reads=[KKt.b, A_.b], writes=[TMP.b])
                    S.op("dve", lambda e: e.tensor_tensor(out=OPS[d].t[:, 2, :], in0=TMP.t[:], in1=ENG_.t[:], op=ALU.mult),
                         reads=[TMP.b, ENG_.b], writes=[OPS[d].b])
                    S.op("dve", lambda e: e.tensor_scalar(out=TMP.t[:], in0=A_.t[:], scalar1=KA_(cb), scalar2=OMKA(cb),
                                                          op0=ALU.mult, op1=ALU.add), reads=[A_.b], writes=[TMP.b])
                    S.op("dve", lambda e: e.tensor_tensor(out=TMP.t[:], in0=TMP.t[:], in1=K_.t[:], op=ALU.mult),
                         reads=[TMP.b, K_.b], writes=[TMP.b])
                    S.op("dve", lambda e: e.tensor_tensor(out=OPS[d].t[:, 3, :], in0=TMP.t[:], in1=ENG_.t[:], op=ALU.mult),
                         reads=[TMP.b, ENG_.b], writes=[OPS[d].b])
                if debug.get("ops_dump") == cb:
                    for d in range(2):
                        for a in range(4):
                            S.op("act", lambda e: e.activation(out=TMP.t[:], in_=OPS[d].t[:, a, :], func=AF.Copy),
                                 reads=[OPS[d].b], writes=[TMP.b])
                            S.dma("sp", dbg[(d * 4 + a) * 128:(d * 4 + a + 1) * 128, :], TMP.t[:], reads=[TMP.b])
                    S.dma("sp", dbg[1024:1152, 0:16], GAM[0].t[:], reads=[GAM[0].b])
                    S.dma("sp", dbg[1152:1280, 0:16], GAM[1].t[:], reads=[GAM[1].b])

                if debug.get("prep_only"):
                    break
                S.op("dve", lambda e: e.memset(Hf.t[:], 0.0), writes=[Hf.b])
                S.op("dve", lambda e: e.memset(Hb.t[:], 0.0), writes=[Hb.b])

                def inv_gen(s):
                    par = s % 2
                    TM, AT, Q = TMr[par], ATr[par], Qr[par]
                    trs = [slice(chunk_of(d, s) * 128, chunk_of(d, s) * 128 + 128) for d in range(2)]
                    srcs = []
                    for d in range(2):
                        srcs += [(OPS[d].t[:, 2, trs[d]], OPS[d].b), (OPS[d].t[:, 3, trs[d]], OPS[d].b), (Vb.t[:, trs[d]], Vb.b)]
                    for i, (ap_, b_) in enumerate(srcs):
                        S.op("pe", lambda e: e.transpose(ptr.t[:, i * 128:(i + 1) * 128], ap_, IDB), reads=[b_],
                             writes=[ptr.b], inc=(i == 5))
                    S.op("act", lambda e: e.activation(out=TM.t[:].rearrange("p a i -> p (a i)"), in_=ptr.t[:, 0:768],
                                                       func=AF.Copy), reads=[ptr.b], writes=[TM.b])
                    yield
                    for d in range(2):
                        for hh in range(2):
                            ii = 2 * d + hh
                            rs = slice(64 * hh, 64 * hh + 64)
                            aT = OPS[d].t[rs, 0, trs[d]]
                            bT = OPS[d].t[rs, 2, trs[d]]
                            kT = OPS[d].t[rs, 3, trs[d]]
                            arT = OPS[d].t[rs, 0:2, trs[d]]
                            xb = pb[hh]
                            xo = d * 256
                            S.op("pe", lambda e: e.matmul(xb.t[:, xo:xo + 128], lhsT=bT, rhs=aT, start=True, stop=True),
                                 reads=[OPS[d].b], writes=[xb.b], inc=False)
                            S.op("pe", lambda e: e.matmul(xb.t[:, xo + 128:xo + 256], lhsT=aT, rhs=bT, start=True, stop=True),
                                 reads=[OPS[d].b], writes=[xb.b], inc=False)
                            ab = pb[2 + hh]
                            S.op("pe", lambda e: e.matmul(ab.t[:, 0:128], lhsT=bT, rhs=OPS[d].t[rs, 1, trs[d]], start=True,
                                                          stop=True), reads=[OPS[d].b], writes=[ab.b], inc=False)
                            S.op("pe", lambda e: e.matmul(ab.t[:, 128:384].rearrange("p (a i) -> p a i", a=2), lhsT=kT, rhs=arT,
                                                          start=True, stop=True), reads=[OPS[d].b], writes=[ab.b], inc=True)
                            S.op("dve", lambda e: e.tensor_tensor(out=AT.t[:, ii, :], in0=ab.t[:, 0:384], in1=M3[d][:, 0:384],
                                                                  op=ALU.mult), reads=[ab.b], writes=[AT.b])
                        yield
                    LV0 = LVr[0]
                    for h2 in range(2):
                        S.op("dve", lambda e: e.tensor_tensor(
                            out=LV0.t[:].rearrange("p (d h) x -> p d h x", h=2)[:, :, h2, :],
                            in0=pb[h2].t[:, :].rearrange("p (d x) -> p d x", d=2),
                            in1=MXX[:, 256:768].rearrange("p (d x) -> p d x", d=2),
                            op=ALU.mult), reads=[pb[h2].b], writes=[LV0.b])
                    S.op("dve", lambda e: e.tensor_tensor(out=Q.t[:], in0=LV0.t[:, :, 0:128],
                                                          in1=ID4.rearrange("p (a i) -> p a i", a=4), op=ALU.add),
                         reads=[LV0.b], writes=[Q.b])
                    yield
                    for n in range(1, 7):
                        LVp, LVn = LVr[(n - 1) % 2], LVr[n % 2]
                        for ii in range(4):
                            lb = pb[ii // 2]
                            lo = (ii % 2) * 256
                            if n < 6:
                                S.op("pe", lambda e: e.matmul(lb.t[:, lo:lo + 128], lhsT=LVp.t[:, ii, 128:256],
                                                              rhs=LVp.t[:, ii, 0:128], start=True, stop=True), reads=[LVp.b],
                                     writes=[lb.b], inc=False)
                            S.op("pe", lambda e: e.matmul(lb.t[:, lo + 128:lo + 256], lhsT=LVp.t[:, ii, 0:128],
                                                          rhs=LVp.t[:, ii, 128:256], start=True, stop=True), reads=[LVp.b],
                                 writes=[lb.b], inc=(ii % 2 == 1))
                        for h2 in range(2):
                            S.op("act", lambda e: e.activation(out=LVn.t[:, 2 * h2:2 * h2 + 2, :].rearrange("p a x -> p (a x)"),
                                                               in_=pb[h2].t[:, :], func=AF.Copy), reads=[pb[h2].b],
                                 writes=[LVn.b])
                        yield
                        for ii in range(4):
                            S.op("pe", lambda e: e.matmul(pb[4].t[:, ii * 128:(ii + 1) * 128], lhsT=LVn.t[:, ii, 128:256],
                                                          rhs=Q.t[:, ii, :], start=True, stop=True), reads=[LVn.b, Q.b],
                                 writes=[pb[4].b], inc=(ii == 3))
                        S.op("dve", lambda e: e.tensor_tensor(out=Q.t[:].rearrange("p a i -> p (a i)"), in0=pb[4].t[:, :],
                                                              in1=Q.t[:].rearrange("p a i -> p (a i)"), op=ALU.add),
                             reads=[pb[4].b, Q.b], writes=[Q.b])
                        yield

                def chain_gen(s):
                    par = s % 2
                    TM, AT, Q = TMr[par], ATr[par], Qr[par]
                    cds = [chunk_of(d, s) for d in range(2)]
                    trs = [slice(c_ * 128, c_ * 128 + 128) for c_ in cds]
                    for d in range(2):
                        for hh in range(2):
                            ii = 2 * d + hh
                            rs = slice(64 * hh, 64 * hh + 64)
                            S.op("pe", lambda e: e.matmul(pw_ap(ii), lhsT=OPS[d].t[rs, 0, trs[d]], rhs=Hb.t[rs, d, :], start=True,
                                                          stop=False), reads=[OPS[d].b, Hb.b], writes=[PW], inc=False)
                            S.op("pe", lambda e: e.matmul(pw_ap(ii), lhsT=AT.t[:, ii, 128:256],
                                                          rhs=TM.t[:, 3 * d + 2, hh * 64:(hh + 1) * 64], start=False, stop=True),
                                 reads=[AT.b, TM.b], writes=[PW], inc=(ii == 3))
                    S.op("act", lambda e: e.activation(out=Wsb.t[:], in_=pb[5].t[:, 0:256], func=AF.Copy), reads=[PW],
                         writes=[Wsb.b])
                    yield
                    for ii in range(4):
                        S.op("pe", lambda e: e.matmul(pu_ap(ii), lhsT=Q.t[:, ii, :], rhs=Wsb.t[:, ii * 64:(ii + 1) * 64],
                                                      start=True, stop=True), reads=[Q.b, Wsb.b], writes=[PU], inc=(ii == 3))
                    S.op("act", lambda e: e.activation(out=Usb.t[:], in_=pb[5].t[:, 256:512], func=AF.Copy), reads=[PU],
                         writes=[Usb.b])
                    yield
                    for d in range(2):
                        for hh in range(2):
                            ii = 2 * d + hh
                            rs = slice(64 * hh, 64 * hh + 64)
                            vtm = TM.t[:, 3 * d + 2, hh * 64:(hh + 1) * 64]
                            S.op("pe", lambda e: e.matmul(py_ap(d, hh), lhsT=Hb.t[rs, d, :], rhs=OPS[d].t[rs, 1, trs[d]],
                                                          start=True, stop=False), reads=[Hb.b, OPS[d].b], writes=[PY], inc=False)
                            S.op("pe", lambda e: e.matmul(py_ap(d, hh), lhsT=Usb.t[:, ii * 64:(ii + 1) * 64],
                                                          rhs=AT.t[:, ii, 0:128], start=False, stop=False), reads=[Usb.b, AT.b],
                                 writes=[PY], inc=False)
                            S.op("pe", lambda e: e.matmul(py_ap(d, hh), lhsT=vtm, rhs=AT.t[:, ii, 256:384], start=False,
                                                          stop=True), reads=[TM.b, AT.b], writes=[PY], inc=False)
                            S.op("pe", lambda e: e.matmul(ph_ap(d, hh), lhsT=TM.t[:, 3 * d, hh * 64:(hh + 1) * 64],
                                                          rhs=Usb.t[:, ii * 64:(ii + 1) * 64], start=True, stop=False),
                                 reads=[TM.b, Usb.b], writes=[PH], inc=False)
                            S.op("pe", lambda e: e.matmul(ph_ap(d, hh), lhsT=TM.t[:, 3 * d + 1, hh * 64:(hh + 1) * 64], rhs=vtm,
                                                          start=False, stop=True), reads=[TM.b], writes=[PH], inc=(ii == 3))
                    for d in range(2):
                        S.op("act", lambda e: e.activation(out=YD[d].t[:, trs[d]], in_=pb[6].t[:, d * 128:(d + 1) * 128],
                                                           func=AF.Copy), reads=[PY], writes=[YD[d].b])
                        S.op("dve", lambda e: e.tensor_tensor(out=Hf.t[:, d, :], in0=pb[6].t[:, 256 + d * 64:256 + (d + 1) * 64],
                                                              in1=Hf.t[:, d, :], op=ALU.add), reads=[PH, Hf.b], writes=[Hf.b])
                        S.op("dve", lambda e: e.tensor_scalar(out=Hf.t[:, d, :], in0=Hf.t[:, d, :],
                                                              scalar1=GAM[d].t[:, cds[d]:cds[d] + 1], scalar2=None, op0=ALU.mult),
                             reads=[Hf.b, GAM[d].b], writes=[Hf.b])
                    S.op("act", lambda e: e.activation(out=Hb.t[:], in_=Hf.t[:], func=AF.Copy), reads=[Hf.b], writes=[Hb.b])
                    yield

                _interleave([inv_gen(0)])
                for s in range(NT):
                    gens = [chain_gen(s)]
                    if s + 1 < NT:
                        gens.append(inv_gen(s + 1))
                    _interleave(gens)
                if debug.get("y_dump") == cb:
                    S.dma("sp", dbg[0:128, :], YD[0].t[:], reads=[YD[0].b])
                    S.dma("sp", dbg[128:256, :], YD[1].t[:], reads=[YD[1].b])

                S.op("dve", lambda e: e.tensor_tensor(out=YD[0].t[:], in0=YD[0].t[:], in1=YD[1].t[:], op=ALU.add),
                     reads=[YD[0].b, YD[1].b], writes=[YD[0].b])
                Y = YD[0]
                for tq in range(4):
                    ts_ = slice(tq * 512, (tq + 1) * 512)
                    bm, bv_, bb, bg = pb[0], pb[1], pb[2], pb[3]
                    S.op("pe", lambda e: e.matmul(bm.t[:, :], lhsT=BO64, rhs=Y.t[:, ts_], start=True, stop=True), reads=[Y.b],
                         writes=[bm.b])
                    S.op("dve", lambda e: e.tensor_tensor(out=TMP.t[:, ts_], in0=Y.t[:, ts_], in1=bm.t[:, :], op=ALU.subtract),
                         reads=[Y.b, bm.b], writes=[TMP.b])
                    S.op("act", lambda e: e.activation(out=TMP2.t[:, ts_], in_=TMP.t[:, ts_], func=AF.Square), reads=[TMP.b],
                         writes=[TMP2.b])
                    S.op("pe", lambda e: e.matmul(bv_.t[:, :], lhsT=BO64, rhs=TMP2.t[:, ts_], start=True, stop=True),
                         reads=[TMP2.b], writes=[bv_.b])
                    S.op("dve", lambda e: e.tensor_scalar(out=TMP2.t[:, ts_], in0=bv_.t[:, :], scalar1=GN_EPS, scalar2=None,
                                                          op0=ALU.add), reads=[bv_.b], writes=[TMP2.b])
                    S.op("act", lambda e: e.activation(out=TMP2.t[:, ts_], in_=TMP2.t[:, ts_], func=AF.Sqrt), reads=[TMP2.b],
                         writes=[TMP2.b])
                    S.op("dve", lambda e: e.reciprocal(out=TMP2.t[:, ts_], in_=TMP2.t[:, ts_]), reads=[TMP2.b], writes=[TMP2.b])
                    S.op("dve", lambda e: e.tensor_tensor(out=TMP.t[:, ts_], in0=TMP.t[:, ts_], in1=TMP2.t[:, ts_], op=ALU.mult),
                         reads=[TMP.b, TMP2.b], writes=[TMP.b])
                    S.op("dve", lambda e: e.tensor_scalar(out=TMP.t[:, ts_], in0=TMP.t[:, ts_], scalar1=LG_(cb), scalar2=LB_(cb),
                                                          op0=ALU.mult, op1=ALU.add), reads=[TMP.b], writes=[TMP.b])
                    S.op("pe", lambda e: e.matmul(bb.t[:, :], lhsT=BO1, rhs=RKt.t[:, ts_], start=True, stop=True), reads=[RKt.b],
                         writes=[bb.b])
                    S.op("dve", lambda e: e.tensor_tensor(out=TMP2.t[:, ts_], in0=V_.t[:, ts_], in1=bb.t[:, :], op=ALU.mult),
                         reads=[V_.b, bb.b], writes=[TMP2.b])
                    S.op("dve", lambda e: e.tensor_tensor(out=TMP.t[:, ts_], in0=TMP.t[:, ts_], in1=TMP2.t[:, ts_], op=ALU.add),
                         reads=[TMP.b, TMP2.b], writes=[TMP.b])
                    S.op("pe", lambda e: e.matmul(bg.t[:, :], lhsT=G2A.t[:, cb * 128:(cb + 1) * 128], rhs=SGD0.t[:, ts_],
                                                  start=True, stop=False), reads=[G2A.b, SGD0.b], writes=[bg.b], inc=False)
                    S.op("pe", lambda e: e.matmul(bg.t[:, :], lhsT=G2B.t[:, cb * 128:(cb + 1) * 128], rhs=SGD1.t[:, ts_],
                                                  start=False, stop=True), reads=[G2B.b, SGD1.b], writes=[bg.b])
                    S.op("dve", lambda e: e.tensor_tensor(out=MIXo.t[:, ts_], in0=TMP.t[:, ts_], in1=bg.t[:, :], op=ALU.mult),
                         reads=[TMP.b, bg.b], writes=[MIXo.b])
                S.dma("sp", mix_scr[cb * 128:(cb + 1) * 128, :], MIXo.t[:], reads=[MIXo.b])
                if debug.get("ncb") == cb + 1:
                    break
            S.barrier()
        sC.close()
        if upto == "S":
            return nc

        IDF2, IOTA2, EBASE2 = CPS.t[:, 0:128], CPS.t[:, 384:768], CPS.t[:, 768:800]
        inv_d = float(1.0 / D)

        def layer_norm(Z, Gt, Bt, st, junk):
            S.op("dve", lambda e: e.memset(st.t[:, 0:2], 0.0), writes=[st.b])
            S.op("act", lambda e: e.activation(out=junk.t[:], in_=Z.t[:], func=AF.Copy, accum_out=st.t[:, 0:1]),
                 reads=[Z.b], writes=[junk.b, st.b])
            S.op("act", lambda e: e.activation(out=junk.t[:], in_=Z.t[:], func=AF.Square, accum_out=st.t[:, 1:2]),
                 reads=[Z.b], writes=[junk.b, st.b])
            S.op("dve", lambda e: e.tensor_scalar(out=st.t[:, 2:3], in0=st.t[:, 0:1], scalar1=inv_d, scalar2=None,
                                                  op0=ALU.mult), reads=[st.b], writes=[st.b])
            S.op("dve", lambda e: e.tensor_tensor(out=st.t[:, 3:4], in0=st.t[:, 2:3], in1=st.t[:, 2:3], op=ALU.mult),
                 reads=[st.b], writes=[st.b])
            S.op("dve", lambda e: e.scalar_tensor_tensor(out=st.t[:, 4:5], in0=st.t[:, 1:2], scalar=inv_d, in1=st.t[:, 3:4],
                                                         op0=ALU.mult, op1=ALU.subtract), reads=[st.b], writes=[st.b])
            S.op("dve", lambda e: e.tensor_scalar(out=st.t[:, 5:6], in0=st.t[:, 4:5], scalar1=LN_EPS, scalar2=None,
                                                  op0=ALU.add), reads=[st.b], writes=[st.b])
            S.op("act", lambda e: e.activation(out=st.t[:, 6:7], in_=st.t[:, 5:6], func=AF.Sqrt), reads=[st.b], writes=[st.b])
            S.op("dve", lambda e: e.reciprocal(out=st.t[:, 7:8], in_=st.t[:, 6:7]), reads=[st.b], writes=[st.b])
            S.op("dve", lambda e: e.tensor_scalar(out=st.t[:, 8:9], in0=st.t[:, 2:3], scalar1=st.t[:, 7:8], scalar2=-1.0,
                                                  op0=ALU.mult, op1=ALU.mult), reads=[st.b], writes=[st.b])
            S.op("act", lambda e: e.activation(out=Z.t[:], in_=Z.t[:], func=AF.Identity, scale=st.t[:, 7:8],
                                               bias=st.t[:, 8:9]), reads=[Z.b, st.b], writes=[Z.b])
            S.op("dve", lambda e: e.tensor_tensor(out=Z.t[:], in0=Z.t[:], in1=Gt.t[:], op=ALU.mult), reads=[Z.b, Gt.b],
                 writes=[Z.b])
            S.op("dve", lambda e: e.tensor_tensor(out=Z.t[:], in0=Z.t[:], in1=Bt.t[:], op=ALU.add), reads=[Z.b, Bt.b],
                 writes=[Z.b])

        sH = ExitStack()
        BGU = sb(sH, "bgu", [128, NE * 32], F32)
        S.dma("sp", BGU.t[:], bgu_t, writes=[BGU.b])
        with ExitStack() as sO:
            WO = sb(sO, "wo", [128, 16, D], BF16)
            w_out_v = w_out.rearrange("(kc p) c -> p kc c", p=128)
            for kq in range(8):
                S.dma("pool", WO.t[:, kq * 2:(kq + 1) * 2, :], w_out_v[:, kq * 2:(kq + 1) * 2, :], writes=[WO.b])
            LNG = sb(sO, "lng", [128, D], F32)
            LNB = sb(sO, "lnb", [128, D], F32)
            S.dma("sp", LNG.t[:], rows[0:1, :].broadcast_to([128, D]), writes=[LNG.b])
            S.dma("sp", LNB.t[:], rows[1:2, :].broadcast_to([128, D]), writes=[LNB.b])
            BR = sb(sO, "br", [128, NE], F32)
            S.dma("sp", BR.t[:], rows[4:5, 0:NE].broadcast_to([128, NE]), writes=[BR.b])
            WR = sb(sO, "wr", [128, 16, NE], F32)
            S.dma("sp", WR.t[:], w_router.rearrange("(kc p) e -> p kc e", p=128), writes=[WR.b])
            MASKB = sb(sO, "maskb", [128, NT, NE], BF16)
            MTr = [sb(sO, "mt%d" % i, [128, 16, 128], BF16) for i in range(2)]
            XTr = [sb(sO, "xt%d" % i, [128, D], F32) for i in range(1)]
            Hr = [sb(sO, "h%d" % i, [128, D], F32) for i in range(2)]
            HBr = [sb(sO, "hb%d" % i, [128, D], BF16) for i in range(2)]
            HT = sb(sO, "ht", [128, 16, 128], F32)
            STr = [sb(sO, "lnst%d" % i, [128, 16], F32) for i in range(2)]
            sm = lambda n, w=NE: sb(sO, n, [128, w], F32)
            Lt, M8, MASKF, NEGM, EX, EM, DEN, GATEt, POS, DALL, OH, T1, JNK, DESTF = (
                sm("rl"), sm("rm8", 8), sm("rmask"), sm("rnegm", 1), sm("rex"), sm("rem"), sm("rden", 2), sm("rgate"),
                sm("rpos"), sm("rdall"), sm("roh"), sm("rt1"), sm("rjnk"), sm("rdestf", 4))
            mix_v = mix_scr.rearrange("(kc p) t -> p kc t", p=128)
            for tt in range(NT):
                trs_ = slice(tt * 128, (tt + 1) * 128)
                MT, XT, H, st = MTr[tt % 2], XTr[0], Hr[tt % 2], STr[tt % 2]
                S.dma("sp", MT.t[:], mix_v[:, :, trs_], writes=[MT.b])
                S.dma("sp", XT.t[:], x_tm[trs_, :], writes=[XT.b])
                for dblk in range(4):
                    bank = pb[dblk]
                    ds_ = slice(dblk * 512, (dblk + 1) * 512)
                    for kc in range(16):
                        S.op("pe", lambda e: e.matmul(bank.t[:, :], lhsT=MT.t[:, kc, :], rhs=WO.t[:, kc, ds_],
                                                      start=(kc == 0), stop=(kc == 15)), reads=[MT.b, WO.b],
                             writes=[bank.b], inc=(kc == 15))
                    S.op("dve", lambda e: e.scalar_tensor_tensor(out=H.t[:, ds_], in0=XT.t[:, ds_], scalar=ALPHA,
                                                                 in1=bank.t[:, :], op0=ALU.mult, op1=ALU.add),
                         reads=[XT.b, bank.b], writes=[H.b])
                layer_norm(H, LNG, LNB, st, HT)
                S.dma("sp", h1_scr[trs_, :], H.t[:], reads=[H.b])
                HB = HBr[tt % 2]
                S.op("act", lambda e: e.activation(out=HB.t[:], in_=H.t[:], func=AF.Copy), reads=[H.b], writes=[HB.b])
                S.dma("sp", h1b_scr[trs_, :], HB.t[:], reads=[HB.b])
                for fq in range(4):
                    tb = pb[4 + fq % 2]
                    for i in range(4):
                        fc = fq * 4 + i
                        S.op("pe", lambda e: e.transpose(tb.t[:, i * 128:(i + 1) * 128], H.t[:, fc * 128:(fc + 1) * 128], IDF2),
                             reads=[H.b], writes=[tb.b], inc=(i == 3))
                    S.op("act", lambda e: e.activation(out=HT.t[:, fq * 4:(fq + 1) * 4, :].rearrange("p a i -> p (a i)"),
                                                       in_=tb.t[:, :], func=AF.Copy), reads=[tb.b], writes=[HT.b])
                lb = pb[6]
                for fc in range(16):
                    S.op("pe", lambda e: e.matmul(lb.t[:, 0:NE], lhsT=HT.t[:, fc, :], rhs=WR.t[:, fc, :], start=(fc == 0),
                                                  stop=(fc == 15)), reads=[HT.b, WR.b], writes=[lb.b], inc=(fc == 15))
                S.op("dve", lambda e: e.tensor_tensor(out=Lt.t[:], in0=lb.t[:, 0:NE], in1=BR.t[:], op=ALU.add),
                     reads=[lb.b, BR.b], writes=[Lt.b])
                S.op("dve", lambda e: e.max(out=M8.t[:], in_=Lt.t[:]), reads=[Lt.b], writes=[M8.b])
                S.op("dve", lambda e: e.tensor_scalar(out=MASKF.t[:], in0=Lt.t[:], scalar1=M8.t[:, 3:4], scalar2=None,
                                                      op0=ALU.is_ge), reads=[Lt.b, M8.b], writes=[MASKF.b])
                S.op("dve", lambda e: e.tensor_copy(out=MASKB.t[:, tt, :], in_=MASKF.t[:]), reads=[MASKF.b], writes=[MASKB.b])
                S.op("dve", lambda e: e.tensor_scalar(out=NEGM.t[:], in0=M8.t[:, 0:1], scalar1=-1.0, scalar2=None,
                                                      op0=ALU.mult), reads=[M8.b], writes=[NEGM.b])
                S.op("act", lambda e: e.activation(out=EX.t[:], in_=Lt.t[:], func=AF.Exp, bias=NEGM.t[:, 0:1]),
                     reads=[Lt.b, NEGM.b], writes=[EX.b])
                S.op("dve", lambda e: e.tensor_tensor(out=EM.t[:], in0=EX.t[:], in1=MASKF.t[:], op=ALU.mult),
                     reads=[EX.b, MASKF.b], writes=[EM.b])
                S.op("dve", lambda e: e.memset(DEN.t[:], 0.0), writes=[DEN.b])
                S.op("act", lambda e: e.activation(out=JNK.t[:], in_=EM.t[:], func=AF.Copy, accum_out=DEN.t[:, 0:1]),
                     reads=[EM.b], writes=[JNK.b, DEN.b])
                S.op("dve", lambda e: e.reciprocal(out=DEN.t[:, 1:2], in_=DEN.t[:, 0:1]), reads=[DEN.b], writes=[DEN.b])
                S.op("dve", lambda e: e.tensor_scalar(out=GATEt.t[:], in0=EM.t[:], scalar1=DEN.t[:, 1:2], scalar2=None,
                                                      op0=ALU.mult), reads=[EM.b, DEN.b], writes=[GATEt.b])
                S.op("pe", lambda e: e.matmul(lb.t[:, NE:2 * NE], lhsT=TRISB, rhs=MASKB.t[:, tt, :], start=True,
                                              stop=(tt == 0)), reads=[MASKB.b], writes=[lb.b], inc=(tt == 0))
                for t2 in range(tt):
                    S.op("pe", lambda e: e.matmul(lb.t[:, NE:2 * NE], lhsT=ONESB, rhs=MASKB.t[:, t2, :], start=False,
                                                  stop=(t2 == tt - 1)), reads=[MASKB.b], writes=[lb.b], inc=(t2 == tt - 1))
                S.op("dve", lambda e: e.tensor_copy(out=POS.t[:], in_=lb.t[:, NE:2 * NE]), reads=[lb.b], writes=[POS.b])
                S.op("dve", lambda e: e.tensor_tensor(out=DALL.t[:], in0=POS.t[:], in1=EBASE2, op=ALU.add), reads=[POS.b],
                     writes=[DALL.b])
                S.op("dve", lambda e: e.scalar_tensor_tensor(out=T1.t[:], in0=POS.t[:], scalar=1.0, in1=MASKF.t[:],
                                                             op0=ALU.add, op1=ALU.mult), reads=[POS.b, MASKF.b],
                     writes=[T1.b])
                S.op("dve", lambda e: e.tensor_scalar(out=POSM.t[:, tt, :], in0=T1.t[:], scalar1=-1.0, scalar2=None,
                                                      op0=ALU.add), reads=[T1.b], writes=[POSM.b])
                S.op("dve", lambda e: e.memset(DESTF.t[:], 0.0), writes=[DESTF.b])
                S.op("dve", lambda e: e.memset(GK.t[:, tt * 4:(tt + 1) * 4], 0.0), writes=[GK.b])
                for k in range(4):
                    S.op("dve", lambda e: e.tensor_scalar(out=OH.t[:], in0=Lt.t[:], scalar1=M8.t[:, k:k + 1], scalar2=None,
                                                          op0=ALU.is_equal), reads=[Lt.b, M8.b], writes=[OH.b])
                    S.op("dve", lambda e: e.tensor_tensor(out=T1.t[:], in0=OH.t[:], in1=DALL.t[:], op=ALU.mult),
                         reads=[OH.b, DALL.b], writes=[T1.b])
                    S.op("act", lambda e: e.activation(out=JNK.t[:], in_=T1.t[:], func=AF.Copy, accum_out=DESTF.t[:, k:k + 1]),
                         reads=[T1.b], writes=[JNK.b, DESTF.b])
                    S.op("dve", lambda e: e.tensor_tensor(out=T1.t[:], in0=OH.t[:], in1=GATEt.t[:], op=ALU.mult),
                         reads=[OH.b, GATEt.b], writes=[T1.b])
                    S.op("act", lambda e: e.activation(out=JNK.t[:], in_=T1.t[:], func=AF.Copy,
                                                       accum_out=GK.t[:, tt * 4 + k:tt * 4 + k + 1]),
                         reads=[T1.b], writes=[JNK.b, GK.b])
                S.op("dve", lambda e: e.tensor_copy(out=DEST.t[:, tt * 4:(tt + 1) * 4], in_=DESTF.t[:]), reads=[DESTF.b],
                     writes=[DEST.b])
                if "dbg" in debug.get("dump", ()):
                    S.op("dve", lambda e: e.tensor_copy(out=OH.t[:, 0:4], in_=DEST.t[:, tt * 4:(tt + 1) * 4]), reads=[DEST.b],
                         writes=[OH.b])
                    S.dma("sp", dbg[trs_, 136:140], OH.t[:, 0:4], reads=[OH.b])
                    for j, X_ in enumerate((Lt, MASKF, GATEt, POS)):
                        S.dma("sp", dbg[trs_, j * NE:(j + 1) * NE], X_.t[:], reads=[X_.b])
                    S.dma("sp", dbg[trs_, 128:132], DESTF.t[:], reads=[DESTF.b])
                    S.dma("sp", dbg[trs_, 132:136], GK.t[:, tt * 4:(tt + 1) * 4], reads=[GK.b])
            S.barrier()
        if upto == "O":
            sH.close()
            return nc

        NEX = debug.get("nex", NE)
        with sH, ExitStack() as sE:
            TIDB = sb(sE, "tidb", [128, NT, 2], BF16)
            TIDF = sb(sE, "tidf", [128, 2 * NT], F32)
            S.dma("sp", TIDF.t[:], tid, writes=[TIDF.b])
            S.op("dve", lambda e: e.tensor_copy(out=TIDB.t[:].rearrange("p a b -> p (a b)"), in_=TIDF.t[:]), reads=[TIDF.b],
                 writes=[TIDB.b])
            PEr = [sb(sE, "pe%d" % i, [128, NT, CAP], BF16) for i in range(1)]
            IDXF = [sb(sE, "idxf%d" % i, [128, NJ], F32) for i in range(2)]
            IDXR = [sb(sE, "idxr%d" % i, [128, 2 * NJ], F32) for i in range(2)]
            IDXs = [sb(sE, "idx%d" % i, [128, NJ], I32) for i in range(NEX)]
            XGr = [[sb(sE, "xg%d_%d" % (i, j), [128, D], BF16) for j in range(NJ)] for i in range(2)]
            XeTr = [sb(sE, "xet%d" % i, [128, 16, CAP], BF16) for i in range(2)]
            NWB = 12
            WBr = [sb(sE, "wb%d" % i, [128, 16, 256], BF16) for i in range(NWB)]
            ACTT = sb(sE, "actt", [128, 16, CAP], BF16)
            G32r = [sb(sE, "g32_%d" % i, [128, CAP], F32) for i in range(2)]
            S32r = [sb(sE, "s32_%d" % i, [128, CAP], F32) for i in range(2)]
            L32r = [sb(sE, "l32_%d" % i, [128, CAP], F32) for i in range(2)]
            BD = sb(sE, "bd", [128, D], F32)
            Yer = [sb(sE, "ye%d" % i, [128, 256], F32) for i in range(4)]
            cnt = {"bk": 0, "ye": 0, "pt": 0, "wb": 0}

            def next_bank():
                b_ = pb[cnt["bk"] % 6]
                cnt["bk"] += 1
                return b_

            def next_wb():
                w_ = WBr[cnt["wb"] % NWB]
                cnt["wb"] += 1
                return w_

            def build_idx(ex):
                PEe = PEr[0]
                for tt in range(NT):
                    S.op("dve", lambda e: e.tensor_scalar(out=PEe.t[:, tt, :], in0=IOTA2, scalar1=POSM.t[:, tt, ex:ex + 1],
                                                          scalar2=None, op0=ALU.is_equal), reads=[POSM.b], writes=[PEe.b])
                ib = pb[6]
                for jc in range(NJ):
                    for tt in range(NT):
                        S.op("pe", lambda e: e.matmul(ib.t[:, jc * 2:(jc + 1) * 2], lhsT=PEe.t[:, tt, jc * 128:(jc + 1) * 128],
                                                      rhs=TIDB.t[:, tt, :], start=(tt == 0), stop=(tt == NT - 1)),
                             reads=[PEe.b, TIDB.b], writes=[ib.b], inc=(tt == NT - 1))
                xf = IDXF[ex % 2]
                xr = IDXR[ex % 2]
                S.op("dve", lambda e: e.tensor_copy(out=xr.t[:], in_=ib.t[:, 0:2 * NJ]), reads=[ib.b], writes=[xr.b])
                iv = xr.t[:].rearrange("p (j c) -> p j c", c=2)
                S.op("dve", lambda e: e.scalar_tensor_tensor(out=xf.t[:], in0=iv[:, :, 1], scalar=128.0, in1=iv[:, :, 0],
                                                             op0=ALU.mult, op1=ALU.add), reads=[xr.b], writes=[xf.b])
                S.op("dve", lambda e: e.tensor_copy(out=IDXs[ex].t[:], in_=xf.t[:]), reads=[xf.b], writes=[IDXs[ex].b])

            def gather(ex):
                for jc in range(NJ):
                    xg = XGr[ex % 2][jc]
                    S.gather(xg.t[:], h1b_scr, IDXs[ex].t[:, jc:jc + 1], reads=[IDXs[ex].b], writes=[xg.b])

            def tr_round(ex, r):
                XeT = XeTr[ex % 2]
                for u in range(2):
                    fc = 2 * r + u
                    for jc in range(NJ):
                        xg = XGr[ex % 2][jc]
                        S.op("pe", lambda e: e.transpose(ptr.t[:, u * CAP + jc * 128:u * CAP + (jc + 1) * 128],
                                                         xg.t[:, fc * 128:(fc + 1) * 128], IDB), reads=[xg.b], writes=[ptr.b],
                             inc=(u == 1 and jc == NJ - 1))
                S.op("act", lambda e: e.activation(out=XeT.t[:, 2 * r:2 * r + 2, :].rearrange("p a j -> p (a j)"),
                                                   in_=ptr.t[:, 0:2 * CAP], func=AF.Copy), reads=[ptr.b], writes=[XeT.b])

            build_idx(0)
            if NEX > 1:
                build_idx(1)
            gather(0)
            for r in range(8):
                tr_round(0, r)
            for ex in range(NEX):
                XeT = XeTr[ex % 2]
                if ex + 2 < NEX:
                    build_idx(ex + 2)
                if ex + 1 < NEX:
                    gather(ex + 1)
                S.dma("sp", BD.t[:], b_down[ex:ex + 1, :].broadcast_to([128, D]), writes=[BD.b])
                wgv = w_gu[ex].rearrange("(kc p) c -> p kc c", p=128)
                for pblk in range(8):
                    WG, WL = next_wb(), next_wb()
                    S.dma("pool", WG.t[:], wgv[:, :, pblk * 256:(pblk + 1) * 256], writes=[WG.b])
                    S.dma("pool", WL.t[:], wgv[:, :, D + pblk * 256:D + (pblk + 1) * 256], writes=[WL.b])
                    for sub in range(2):
                        pt = pblk * 2 + sub
                        bg_, bl_ = next_bank(), next_bank()
                        g32, s32, l32 = G32r[cnt["pt"] % 2], S32r[cnt["pt"] % 2], L32r[cnt["pt"] % 2]
                        cnt["pt"] += 1
                        for kc in range(16):
                            S.op("pe", lambda e: e.matmul(bg_.t[:, 0:CAP], lhsT=WG.t[:, kc, sub * 128:(sub + 1) * 128],
                                                          rhs=XeT.t[:, kc, :], start=(kc == 0), stop=(kc == 15)),
                                 reads=[WG.b, XeT.b], writes=[bg_.b], inc=(kc == 15))
                        for kc in range(16):
                            S.op("pe", lambda e: e.matmul(bl_.t[:, 0:CAP], lhsT=WL.t[:, kc, sub * 128:(sub + 1) * 128],
                                                          rhs=XeT.t[:, kc, :], start=(kc == 0), stop=(kc == 15)),
                                 reads=[WL.b, XeT.b], writes=[bl_.b], inc=(kc == 15))
                        cg = ex * 32 + pt
                        cl = ex * 32 + 16 + pt
                        S.op("dve", lambda e: e.tensor_scalar(out=g32.t[:], in0=bg_.t[:, 0:CAP], scalar1=BGU.t[:, cg:cg + 1],
                                                              scalar2=7.0, op0=ALU.add, op1=ALU.min), reads=[bg_.b, BGU.b],
                             writes=[g32.b])
                        S.op("act", lambda e: e.activation(out=s32.t[:], in_=g32.t[:], func=AF.Sigmoid, scale=1.702),
                             reads=[g32.b], writes=[s32.b])
                        S.op("dve", lambda e: e.tensor_scalar(out=l32.t[:], in0=bl_.t[:, 0:CAP], scalar1=BGU.t[:, cl:cl + 1],
                                                              scalar2=7.0, op0=ALU.add, op1=ALU.min), reads=[bl_.b, BGU.b],
                             writes=[l32.b])
                        S.op("dve", lambda e: e.tensor_scalar(out=l32.t[:], in0=l32.t[:], scalar1=-7.0, scalar2=1.0,
                                                              op0=ALU.max, op1=ALU.add), reads=[l32.b], writes=[l32.b])
                        S.op("dve", lambda e: e.tensor_tensor(out=g32.t[:], in0=g32.t[:], in1=s32.t[:], op=ALU.mult),
                             reads=[g32.b, s32.b], writes=[g32.b])
                        S.op("dve", lambda e: e.tensor_tensor(out=ACTT.t[:, pt, :], in0=g32.t[:], in1=l32.t[:], op=ALU.mult),
                             reads=[g32.b, l32.b], writes=[ACTT.b])
                        if ex + 1 < NEX and 4 <= pt < 12:
                            tr_round(ex + 1, pt - 4)
                wdv = w_down[ex].rearrange("(kc p) c -> p kc c", p=128)
                for dblk in range(8):
                    WD = next_wb()
                    dsl = slice(dblk * 256, (dblk + 1) * 256)
                    S.dma("pool", WD.t[:], wdv[:, :, dsl], writes=[WD.b])
                    for jc in range(NJ):
                        bank = next_bank()
                        ye = Yer[cnt["ye"] % 4]
                        cnt["ye"] += 1
                        for fc in range(16):
                            S.op("pe", lambda e: e.matmul(bank.t[:, 0:256], lhsT=ACTT.t[:, fc, jc * 128:(jc + 1) * 128],
                                                          rhs=WD.t[:, fc, :], start=(fc == 0), stop=(fc == 15)),
                                 reads=[ACTT.b, WD.b], writes=[bank.b], inc=(fc == 15))
                        S.op("dve", lambda e: e.tensor_tensor(out=ye.t[:], in0=bank.t[:, 0:256], in1=BD.t[:, dsl], op=ALU.add),
                             reads=[bank.b, BD.b], writes=[ye.b])
                        S.dma("sp", y_scr[ex * CAP + jc * 128:ex * CAP + (jc + 1) * 128, dsl], ye.t[:], reads=[ye.b])
            S.barrier()
        if upto == "E":
            return nc

        with ExitStack() as sM:
            LNG2 = sb(sM, "lng2", [128, D], F32)
            LNB2 = sb(sM, "lnb2", [128, D], F32)
            S.dma("sp", LNG2.t[:], rows[2:3, :].broadcast_to([128, D]), writes=[LNG2.b])
            S.dma("sp", LNB2.t[:], rows[3:4, :].broadcast_to([128, D]), writes=[LNB2.b])
            H1r = [sb(sM, "h1_%d" % i, [128, D], F32) for i in range(2)]
            Ykr = [sb(sM, "yk%d" % i, [128, D], F32) for i in range(8)]
            ACr = [sb(sM, "acc%d" % i, [128, D], F32) for i in range(2)]
            JK2 = sb(sM, "jk2", [128, D], F32)
            ST2 = [sb(sM, "ln2st%d" % i, [128, 16], F32) for i in range(2)]
            for tt in range(NT):
                trs_ = slice(tt * 128, (tt + 1) * 128)
                H1, AC, st = H1r[tt % 2], ACr[tt % 2], ST2[tt % 2]
                S.dma("sp", H1.t[:], h1_scr[trs_, :], writes=[H1.b])
                yks = [Ykr[(tt % 2) * 4 + k] for k in range(4)]
                for k in range(4):
                    c = tt * 4 + k
                    S.gather(yks[k].t[:], y_scr, DEST.t[:, c:c + 1], reads=[DEST.b], writes=[yks[k].b])
                S.op("act", lambda e: e.activation(out=AC.t[:], in_=H1.t[:], func=AF.Identity, scale=ALPHA), reads=[H1.b],
                     writes=[AC.b])
                for k in range(4):
                    c = tt * 4 + k
                    S.op("dve", lambda e: e.scalar_tensor_tensor(out=AC.t[:], in0=yks[k].t[:], scalar=GK.t[:, c:c + 1],
                                                                 in1=AC.t[:], op0=ALU.mult, op1=ALU.add),
                         reads=[yks[k].b, GK.b, AC.b], writes=[AC.b])
                layer_norm(AC, LNG2, LNB2, st, JK2)
                S.dma("sp", out[trs_, :], AC.t[:], reads=[AC.b])
            S.barrier()
    return nc


def _shared_maps(inp):
    f = lambda a: np.ascontiguousarray(np.asarray(a, dtype=np.float32))
    col = lambda v: f(np.asarray(v).reshape(-1, 128).T)
    mu = np.zeros(28 * 128, np.float32)
    mu[:D_SHIFT] = np.asarray(inp["mu_shift"])[0]
    vecs = np.concatenate([col(inp["a0"][0]), col(inp["k_k"][0]), col(inp["k_a"][0]), col(inp["r_k"][0]),
                           col(inp["lnx_g"][0]), col(inp["lnx_b"][0]), col(inp["beta_f"][0]),
                           np.zeros((128, 8), np.float32)], 1)
    rows = np.zeros((6, D), np.float32)
    rows[0], rows[1] = inp["ln1_g"][0], inp["ln1_b"][0]
    rows[2], rows[3] = inp["ln2_g"][0], inp["ln2_b"][0]
    rows[4, :NE] = inp["b_router"][0]
    w2e = np.concatenate([np.asarray(inp["w2"][0]), np.asarray(inp["w0"][0])[:, None, :]], 1)
    tt = (np.arange(T)[:, None] * np.arange(T)[None, :]) % T
    ang = (2.0 * np.pi / T) * tt.astype(np.float64)
    m = {
        "w_in": f(inp["w_in"][0]), "mu_t": col(mu), "w2e": f(w2e), "a2": f(inp["a2"][0]), "g2": f(inp["g2"][0]),
        "vecs": f(vecs), "w_out": f(inp["w_out"][0]), "rows": rows, "w_router": f(inp["w_router"][0]),
        "w_gu": f(inp["w_gu"][0]),
        "bgu_t": f(np.asarray(inp["b_gu"][0]).reshape(NE, 32, 128).transpose(2, 0, 1).reshape(128, NE * 32)),
        "w_down": f(inp["w_down"][0]), "b_down": f(inp["b_down"][0]),
        "ctab": np.cos(ang).astype(np.float32), "stab": (-np.sin(ang)).astype(np.float32), "cst": _consts(),
        "tid": np.ascontiguousarray(np.stack([np.broadcast_to(np.arange(128, dtype=np.float32)[:, None], (128, NT)),
                                              np.broadcast_to(np.arange(NT, dtype=np.float32)[None, :], (128, NT))],
                                             2).reshape(128, 2 * NT)),
    }
    return m


def _core_maps(inp, shared, b):
    xb = np.asarray(inp["x"][b], dtype=np.float32)
    m = dict(shared)
    m["xT"] = np.ascontiguousarray(xb.T)
    m["x_tm"] = np.ascontiguousarray(xb)
    return m


def kernel(**inputs):
    nc = build()
    shared = _shared_maps(inputs)
    in_maps = [_core_maps(inputs, shared, b) for b in range(8)]
    res = run_bass_kernel_spmd(nc, in_maps, core_ids=list(range(8)))
    return np.stack([np.asarray(r["out"], dtype=np.float32) for r in res.results], 0)
```

```python
import numpy as np
from contextlib import ExitStack
import concourse.bass as bass
import concourse.mybir as mybir
from concourse.bass_utils import run_bass_kernel_spmd

F32 = mybir.dt.float32
BF16 = mybir.dt.bfloat16
I32 = mybir.dt.int32
AF = mybir.ActivationFunctionType
ALU = mybir.AluOpType

T = 2048
D = 2048
NT = 16
NE = 32
CAP = 384
NJ = CAP // 128
KAPPA = -float(np.exp(-0.5))
ALPHA = float(2.0 ** 0.25)
LN_EPS = 1e-5
GN_EPS = 64e-5
D_SHIFT = 3488
D_IN = 4512
ENG = ("pe", "act", "dve", "pool", "sp")
NDMA = 8


class Buf:
    __slots__ = ("w", "r", "const", "excl")

    def __init__(self, const=False):
        self.w = {}
        self.r = {}
        self.const = const
        self.excl = False


class Tl:
    def __init__(self, t, const=False):
        self.t = t
        self.b = Buf(const)


class Sched:
    def __init__(self, nc, es):
        self.nc = nc
        self.e = {"pe": nc.tensor, "act": nc.scalar, "dve": nc.vector, "pool": nc.gpsimd, "sp": nc.sync}
        self.sem = {k: es.enter_context(nc.semaphore("s_" + k)) for k in ENG}
        self.cnt = {k: 0 for k in ENG}
        self.pend = {k: False for k in ENG}
        self.dsem = {q: [es.enter_context(nc.semaphore("d_%s%d" % (q, i))) for i in range(NDMA)]
                     for q in ("sp", "act", "pool")}
        self.dcnt = {q: 0 for q in self.dsem}
        self.waited = {k: {} for k in ENG}

    def _wait(self, eng, evs):
        w = self.waited[eng]
        for sem, val in evs:
            if w.get(sem, 0) >= val:
                continue
            self.e[eng].wait_ge(sem, val)
            w[sem] = val

    def _deps(self, eng, reads, writes, is_dma):
        own = self.sem[eng]
        evs = []
        for b in reads:
            for sem, val in b.w.items():
                if sem is own and not is_dma and eng == "pe":
                    continue
                evs.append((sem, val))
        for b in writes:
            for sem, val in b.w.items():
                if sem is own and not is_dma:
                    continue
                evs.append((sem, val))
            for sem, val in b.r.items():
                if sem is own and not is_dma:
                    continue
                evs.append((sem, val))
        return evs

    def _record(self, ev, reads, writes, merge=False):
        sem, val = ev
        for b in reads:
            if not b.const:
                b.r[sem] = max(b.r.get(sem, 0), val)
        for b in writes:
            if merge:
                b.w = dict(b.w)
                b.w[sem] = max(b.w.get(sem, 0), val)
            else:
                b.w = {sem: val}
            b.r = {}

    def op(self, eng, fn, reads=(), writes=(), inc=True):
        if any(b.excl for b in reads):
            writes = list(writes) + [b for b in reads if b.excl]
            reads = [b for b in reads if not b.excl]
        self._wait(eng, self._deps(eng, reads, writes, False))
        ins = fn(self.e[eng])
        if inc:
            self.cnt[eng] += 1
            ins.then_inc(self.sem[eng], 1)
            ev = (self.sem[eng], self.cnt[eng])
            self.pend[eng] = False
        else:
            ev = (self.sem[eng], self.cnt[eng] + 1)
            self.pend[eng] = True
        self._record(ev, reads, writes)

    def _dma_ev(self, q):
        k = self.dcnt[q]
        self.dcnt[q] += 1
        sem = self.dsem[q][k % NDMA]
        pre = [(sem, 16 * (k // NDMA))] if k >= NDMA else []
        return sem, 16 * (k // NDMA + 1), pre

    def dma(self, q, out, in_, reads=(), writes=()):
        sem, tgt, pre = self._dma_ev(q)
        self._wait(q, self._deps(q, reads, writes, True) + pre)
        self.e[q].dma_start(out=out, in_=in_).then_inc(sem, 16)
        self._record((sem, tgt), reads, writes, merge=True)

    def gather(self, out, table, idx_ap, reads=(), writes=()):
        q = "pool"
        sem, tgt, pre = self._dma_ev(q)
        self._wait(q, self._deps(q, reads, writes, True) + pre)
        self.e[q].indirect_dma_start(out=out, out_offset=None, in_=table,
                                     in_offset=bass.IndirectOffsetOnAxis(ap=idx_ap, axis=0)).then_inc(sem, 16)
        self._record((sem, tgt), reads, writes)

    def barrier(self):
        evs = []
        for k in ENG:
            assert not self.pend[k]
            if self.cnt[k] > 0:
                evs.append((self.sem[k], self.cnt[k]))
        for q, sems in self.dsem.items():
            n = self.dcnt[q]
            for i, sem in enumerate(sems):
                ni = (n - i + NDMA - 1) // NDMA if n > i else 0
                if ni > 0:
                    evs.append((sem, 16 * ni))
        for k in ENG:
            self._wait(k, [ev for ev in evs if ev[0] is not self.sem[k]])


def _interleave(gens):
    gens = list(gens)
    while gens:
        for g in list(gens):
            try:
                next(g)
            except StopIteration:
                gens.remove(g)


def _cst_layout():
    names = [("TRI0", 256), ("TRI1", 256), ("IDF", 128), ("BO64", 128), ("BO1", 128), ("IOTA", 384), ("EBASE", 32),
             ("TRIS", 128), ("CSD", 256), ("MXX", 1024), ("M30", 768), ("M31", 768)]
    off = {}
    o = 0
    for n, w in names:
        off[n] = (o, o + w)
        o += w
    return off, o


CST_OFF, CST_N = _cst_layout()


def _consts():
    c = np.zeros((128, CST_N), np.float32)
    p = np.arange(128)[:, None]
    q = np.arange(128)[None, :]

    def put(name, arr):
        a, b = CST_OFF[name]
        c[:, a:b] = arr

    put("TRI0", np.concatenate([(p <= q) * KAPPA, (p < q) * KAPPA], 1))
    put("TRI1", np.concatenate([(p >= q) * KAPPA, (p > q) * KAPPA], 1))
    put("IDF", (p == q) * 1.0)
    put("BO64", ((p // 64) == (q // 64)) / 64.0)
    put("BO1", ((p // 64) == (q // 64)) * 1.0)
    put("IOTA", np.broadcast_to(np.arange(384)[None, :], (128, 384)))
    put("EBASE", np.broadcast_to((np.arange(32) * CAP)[None, :], (128, 32)))
    put("TRIS", (p < q) * 1.0)
    cc = np.cos(2 * np.pi * (p % 64) * (q % 64) / 64.0) * ((p // 64) == (q // 64))
    ss = np.sin(2 * np.pi * (p % 64) * (q % 64) / 64.0) * ((p // 64) == (q // 64))
    put("CSD", np.concatenate([cc, ss], 1))
    xt0, x0 = (q > p) * 1.0, (q < p) * 1.0
    xt1, x1 = (q < p) * 1.0, (q > p) * 1.0
    put("MXX", np.concatenate([xt0, x0, xt0, x0, xt1, x1, xt1, x1], 1))
    in0, st0 = (q >= p) * 1.0, (q > p) * 1.0
    in1, st1 = (q <= p) * 1.0, (q < p) * 1.0
    put("M30", np.concatenate([in0, st0, in0, in0, st0, in0], 1))
    put("M31", np.concatenate([in1, st1, in1, in1, st1, in1], 1))
    return c


def build(debug=None):
    nc = bass.Bass("TRN2", target_bir_lowering=False)
    debug = debug or {}
    upto = debug.get("upto", "Z")
    ORDER = "AFBSOECZ"
    at_least = lambda st: ORDER.index(upto) >= ORDER.index(st)

    def din(name, shape, dt=F32):
        return nc.dram_tensor(name, list(shape), dt, kind="ExternalInput").ap()

    def dscr(name, shape, dt=F32):
        kind = "ExternalOutput" if name in debug.get("dump", ()) else "Internal"
        return nc.dram_tensor(name, list(shape), dt, kind=kind).ap()

    xT = din("xT", [D, T])
    x_tm = din("x_tm", [T, D])
    w_in = din("w_in", [D, D_IN])
    mu_t = din("mu_t", [128, 28])
    w2e = din("w2e", [2, 65, 1024])
    a2 = din("a2", [2, 64, 1024])
    g2 = din("g2", [160, 1024])
    vecs = din("vecs", [128, 72])
    w_out = din("w_out", [D, D])
    rows = din("rows", [6, D])
    w_router = din("w_router", [D, NE])
    w_gu = din("w_gu", [NE, D, 2 * D]) if at_least("E") else None
    bgu_t = din("bgu_t", [128, NE * 32])
    w_down = din("w_down", [NE, D, D]) if at_least("E") else None
    b_down = din("b_down", [NE, D])
    ctab = din("ctab", [T, T])
    stab = din("stab", [T, T])
    cst = din("cst", [128, CST_N])
    tid = din("tid", [128, 2 * NT])
    out = nc.dram_tensor("out", [T, D], F32, kind="ExternalOutput").ap()

    ps_scr = dscr("ps_scr", [3584, T])
    sg_scr = dscr("sg_scr", [2, T, 1024])
    a_scr = dscr("a_scr", [2, 1024, T])
    mix_scr = dscr("mix_scr", [D, T], BF16)
    h1_scr = dscr("h1_scr", [T, D])
    h1b_scr = dscr("h1b_scr", [T, D], BF16)
    y_scr = dscr("y_scr", [NE * CAP, D])
    dbg = dscr("dbg", [T, D])

    es = ExitStack()
    with es:
        S = Sched(nc, es)

        def sb(stack, name, shape, dt=F32, const=False):
            return Tl(stack.enter_context(nc.sbuf_tensor(name, list(shape), dt)), const)

        pb = [Tl(es.enter_context(nc.psum_tensor("pb%d" % i, [128, 512], F32))) for i in range(7)]
        ptr = Tl(es.enter_context(nc.psum_tensor("ptr", [128, 1024], BF16)))
        for tl in pb + [ptr]:
            tl.b.excl = True

        CPS = sb(es, "cst2_sb", [128, 800], F32)
        S.dma("sp", CPS.t[:], cst[:, 512:1312], writes=[CPS.b])
        POSM = sb(es, "posm", [128, NT, NE], F32)
        DEST = sb(es, "dest", [128, NT * 4], I32)
        GK = sb(es, "gk", [128, NT * 4], F32)
        CB = sb(es, "cst_bf", [128, 1280], BF16)
        VEC = sb(es, "vecs_sb", [128, 72], F32)
        VX = sb(es, "vecx_sb", [128, 16], F32)
        sC = ExitStack()
        C = sb(sC, "cst_sb", [128, CST_N], F32)
        S.dma("sp", C.t[:], cst, writes=[C.b])
        cs = lambda n: C.t[:, CST_OFF[n][0]:CST_OFF[n][1]]
        TRI = [cs("TRI0"), cs("TRI1")]
        IDF, BO64, BO1, IOTA, EBASE = cs("IDF"), cs("BO64"), cs("BO1"), cs("IOTA"), cs("EBASE")
        MXX, M3 = cs("MXX"), [cs("M30"), cs("M31")]
        S.op("dve", lambda e: e.tensor_copy(out=CB.t[:, 0:128], in_=IDF), reads=[C.b], writes=[CB.b])
        for i in range(4):
            S.op("dve", lambda e: e.tensor_copy(out=CB.t[:, 128 + 128 * i:256 + 128 * i], in_=IDF), reads=[C.b],
                 writes=[CB.b])
        S.op("dve", lambda e: e.tensor_copy(out=CB.t[:, 640:768], in_=cs("TRIS")), reads=[C.b], writes=[CB.b])
        S.op("dve", lambda e: e.memset(CB.t[:, 768:1024], 1.0), writes=[CB.b])
        S.op("dve", lambda e: e.tensor_copy(out=CB.t[:, 1024:1280], in_=cs("CSD")), reads=[C.b], writes=[CB.b])
        IDB, ID4, TRISB, ONESB, CSDB = CB.t[:, 0:128], CB.t[:, 128:640], CB.t[:, 640:768], CB.t[:, 768:896], CB.t[:, 1024:1280]
        S.dma("sp", VEC.t[:], vecs, writes=[VEC.b])
        S.op("dve", lambda e: e.tensor_scalar(out=VX.t[:, 0:8], in0=VEC.t[:, 24:32], scalar1=-1.0, scalar2=1.0,
                                              op0=ALU.mult, op1=ALU.add), reads=[VEC.b], writes=[VX.b])
        S.op("dve", lambda e: e.tensor_scalar(out=VX.t[:, 8:16], in0=VEC.t[:, 56:64],
                                              scalar1=float(1.0 / np.sqrt(T * 64.0)), scalar2=None, op0=ALU.mult),
             reads=[VEC.b], writes=[VX.b])
        S.barrier()
        for tl in (C, CPS, CB, VEC, VX):
            tl.b.const = True
        A0 = lambda d, cb: VEC.t[:, d * 8 + cb:d * 8 + cb + 1]
        KK_ = lambda cb: VEC.t[:, 16 + cb:17 + cb]
        KA_ = lambda cb: VEC.t[:, 24 + cb:25 + cb]
        RK_ = lambda cb: VEC.t[:, 32 + cb:33 + cb]
        LG_ = lambda cb: VEC.t[:, 40 + cb:41 + cb]
        LB_ = lambda cb: VEC.t[:, 48 + cb:49 + cb]
        OMKA = lambda cb: VX.t[:, cb:cb + 1]
        BFS = lambda cb: VX.t[:, 8 + cb:9 + cb]

        def finish():
            S.barrier()

        sF = ExitStack()
        fT = sb(sF, "fT", [128, 8, T], BF16)
        with ExitStack() as sA:
            xTb = sb(sA, "xTb", [128, 16, T], BF16)
            for kc in range(16):
                S.dma("pool", xTb.t[:, kc, :], xT[kc * 128:(kc + 1) * 128, :], writes=[xTb.b])
            MU = sb(sA, "mu_sb", [128, 28], F32)
            C1 = sb(sA, "c1_sb", [128, 28], F32)
            C2 = sb(sA, "c2_sb", [128, 28], F32)
            S.dma("sp", MU.t[:], mu_t, writes=[MU.b])
            S.op("dve", lambda e: e.tensor_scalar(out=C1.t[:], in0=MU.t[:], scalar1=-1.0, scalar2=1.0, op0=ALU.mult,
                                                  op1=ALU.add), reads=[MU.b], writes=[C1.b])
            S.op("dve", lambda e: e.tensor_scalar(out=C2.t[:], in0=MU.t[:], scalar1=0.5, scalar2=None, op0=ALU.mult),
                 reads=[MU.b], writes=[C2.b])
            wring = [sb(sA, "wblk%d" % i, [128, 16, 128], BF16) for i in range(2)]
            Pr = [sb(sA, "Pr%d" % i, [128, T], F32) for i in range(2)]
            Sx = sb(sA, "Sx", [128, T], F32)
            Or = [sb(sA, "Or%d" % i, [128, T], F32) for i in range(2)]
            blocks = [(c0, 128) for c0 in range(0, 3328, 128)] + [(3328, 128), (3456, 32)] + \
                     [(D_SHIFT + 128 * i, 128) for i in range(8)]
            w_in_v = w_in.rearrange("(kc p) c -> p kc c", p=128)
            nbank = 0
            for bi, (c0, n) in enumerate(blocks):
                wb = wring[bi % 2]
                S.dma("pool", wb.t[:, :, 0:n], w_in_v[:, :, c0:c0 + n], writes=[wb.b])
                isf = bi >= 28
                P = Pr[bi % 2]
                for tq in range(4):
                    bank = pb[nbank % 4]
                    nbank += 1
                    for kc in range(16):
                        S.op("pe", lambda e: e.matmul(bank.t[0:n, :], lhsT=wb.t[:, kc, 0:n],
                                                      rhs=xTb.t[:, kc, tq * 512:(tq + 1) * 512],
                                                      start=(kc == 0), stop=(kc == 15)),
                             reads=[wb.b, xTb.b], writes=[bank.b], inc=(kc == 15))
                    if isf:
                        S.op("act", lambda e: e.activation(out=fT.t[:, bi - 28, tq * 512:(tq + 1) * 512],
                                                           in_=bank.t[:, :], func=AF.Copy),
                             reads=[bank.b], writes=[fT.b])
                    else:
                        S.op("act", lambda e: e.activation(out=P.t[0:n, tq * 512:(tq + 1) * 512], in_=bank.t[0:n, :],
                                                           func=AF.Copy), reads=[bank.b], writes=[P.b])
                if isf:
                    continue
                O = Or[bi % 2]
                S.op("dve", lambda e: e.tensor_tensor(out=Sx.t[0:n, 1:T - 1], in0=P.t[0:n, 0:T - 2], in1=P.t[0:n, 2:T],
                                                      op=ALU.add), reads=[P.b], writes=[Sx.b])
                S.op("dve", lambda e: e.tensor_copy(out=Sx.t[0:n, 0:1], in_=P.t[0:n, 1:2]), reads=[P.b], writes=[Sx.b])
                S.op("dve", lambda e: e.tensor_copy(out=Sx.t[0:n, T - 1:T], in_=P.t[0:n, T - 2:T - 1]), reads=[P.b],
                     writes=[Sx.b])
                S.op("dve", lambda e: e.tensor_scalar(out=Sx.t[0:n, :], in0=Sx.t[0:n, :], scalar1=C2.t[0:n, bi:bi + 1],
                                                      scalar2=None, op0=ALU.mult), reads=[Sx.b, C2.b], writes=[Sx.b])
                S.op("dve", lambda e: e.scalar_tensor_tensor(out=O.t[0:n, :], in0=P.t[0:n, :], scalar=C1.t[0:n, bi:bi + 1],
                                                             in1=Sx.t[0:n, :], op0=ALU.mult, op1=ALU.add),
                     reads=[P.b, Sx.b, C1.b], writes=[O.b])
                S.dma("sp", ps_scr[c0:c0 + n, :], O.t[0:n, :], reads=[O.b])
            S.barrier()
        if upto == "A":
            sF.close()
            sC.close()
            return nc

        with sF:
            Fc = sb(sF, "Fc", [128, NT, 1024], BF16)
            Fs = sb(sF, "Fs", [128, NT, 1024], BF16)
            for tt in range(NT):
                bc = [pb[0], pb[1]]
                bs_ = [pb[2], pb[3]]
                for blk in range(8):
                    S.op("pe", lambda e: e.matmul(bc[blk // 4].t[:, (blk % 4) * 128:(blk % 4 + 1) * 128],
                                                  lhsT=fT.t[:, blk, tt * 128:(tt + 1) * 128], rhs=CSDB[:, 0:128],
                                                  start=True, stop=True), reads=[fT.b, CB.b], writes=[bc[blk // 4].b],
                         inc=False)
                    S.op("pe", lambda e: e.matmul(bs_[blk // 4].t[:, (blk % 4) * 128:(blk % 4 + 1) * 128],
                                                  lhsT=fT.t[:, blk, tt * 128:(tt + 1) * 128], rhs=CSDB[:, 128:256],
                                                  start=True, stop=True), reads=[fT.b, CB.b], writes=[bs_[blk // 4].b],
                         inc=(blk == 7))
                for h in range(2):
                    S.op("act", lambda e: e.activation(out=Fc.t[:, tt, h * 512:(h + 1) * 512], in_=bc[h].t[:, :],
                                                       func=AF.Copy), reads=[bc[h].b], writes=[Fc.b])
                    S.op("dve", lambda e: e.tensor_copy(out=Fs.t[:, tt, h * 512:(h + 1) * 512], in_=bs_[h].t[:, :]),
                         reads=[bs_[h].b], writes=[Fs.b])
            ctv = ctab.rearrange("(tt p) q -> p tt q", p=128)
            stv = stab.rearrange("(tt p) q -> p tt q", p=128)
            CTr = [sb(sF, "CT%d" % i, [128, NT, 512], BF16) for i in range(2)]
            STr = [sb(sF, "ST%d" % i, [128, NT, 512], BF16) for i in range(2)]
            Mo = [sb(sF, "Mo%d" % i, [128, 512], BF16) for i in range(2)]
            nb = 0
            for tq in range(4):
                CT, ST = CTr[tq % 2], STr[tq % 2]
                S.dma("pool", CT.t[:], ctv[:, :, tq * 512:(tq + 1) * 512], writes=[CT.b])
                S.dma("pool", ST.t[:], stv[:, :, tq * 512:(tq + 1) * 512], writes=[ST.b])
                for blk in range(8):
                    bank = pb[nb % 4]
                    mo = Mo[nb % 2]
                    nb += 1
                    for tt in range(NT):
                        S.op("pe", lambda e: e.matmul(bank.t[:, :], lhsT=Fc.t[:, tt, blk * 128:(blk + 1) * 128],
                                                      rhs=CT.t[:, tt, :], start=(tt == 0), stop=False),
                             reads=[Fc.b, CT.b], writes=[bank.b], inc=False)
                        S.op("pe", lambda e: e.matmul(bank.t[:, :], lhsT=Fs.t[:, tt, blk * 128:(blk + 1) * 128],
                                                      rhs=ST.t[:, tt, :], start=False, stop=(tt == NT - 1)),
                             reads=[Fs.b, ST.b], writes=[bank.b], inc=(tt == NT - 1))
                    S.op("act", lambda e: e.activation(out=mo.t[:, :], in_=bank.t[:, :], func=AF.Identity,
                                                       scale=BFS(blk)), reads=[bank.b], writes=[mo.b])
                    S.dma("sp", mix_scr[1024 + blk * 128:1024 + (blk + 1) * 128, tq * 512:(tq + 1) * 512], mo.t[:, :],
                          reads=[mo.b])
            S.barrier()
        if upto == "F":
            sC.close()
            return nc

        sG = ExitStack()
        SGD0 = sb(sG, "sgd0", [128, T], BF16)
        SGD1 = sb(sG, "sgd1", [32, T], BF16)
        G2A = sb(sG, "g2a", [128, 1024], BF16)
        G2B = sb(sG, "g2b", [32, 1024], BF16)
        S.dma("pool", G2A.t[:], g2[0:128, :], writes=[G2A.b])
        S.dma("pool", G2B.t[:], g2[128:160, :], writes=[G2B.b])
        with ExitStack() as sB:
            stg = sb(sB, "stg", [128, T], F32)
            S.dma("sp", stg.t[:], ps_scr[3328:3456, :], writes=[stg.b])
            S.op("act", lambda e: e.activation(out=SGD0.t[:], in_=stg.t[:], func=AF.Sigmoid), reads=[stg.b], writes=[SGD0.b])
            stg2 = sb(sB, "stg2", [32, T], F32)
            S.dma("sp", stg2.t[:], ps_scr[3456:3488, :], writes=[stg2.b])
            S.op("act", lambda e: e.activation(out=SGD1.t[:], in_=stg2.t[:], func=AF.Sigmoid), reads=[stg2.b], writes=[SGD1.b])
            nb = 0
            for d in range(2):
                wdt = sb(sB, "wdt%d" % d, [65, T], F32)
                w2s = sb(sB, "w2s%d" % d, [65, 1024], F32)
                S.dma("sp", wdt.t[0:64, :], ps_scr[3072 + 64 * d:3136 + 64 * d, :], writes=[wdt.b])
                S.dma("sp", w2s.t[:], w2e[d], writes=[w2s.b])
                S.op("act", lambda e: e.activation(out=wdt.t[0:64, :], in_=wdt.t[0:64, :], func=AF.Tanh), reads=[wdt.b],
                     writes=[wdt.b])
                S.op("dve", lambda e: e.memset(wdt.t[64:65, :], 1.0), writes=[wdt.b])
                SGt = [sb(sB, "sgt%d_%d" % (d, i), [128, 1024], F32) for i in range(2)]
                for tt in range(NT):
                    sg = SGt[tt % 2]
                    for h in range(2):
                        bank = pb[nb % 4]
                        nb += 1
                        S.op("pe", lambda e: e.matmul(bank.t[:, :], lhsT=wdt.t[:, tt * 128:(tt + 1) * 128],
                                                      rhs=w2s.t[:, h * 512:(h + 1) * 512], start=True, stop=True),
                             reads=[wdt.b, w2s.b], writes=[bank.b])
                        S.op("act", lambda e: e.activation(out=sg.t[:, h * 512:(h + 1) * 512], in_=bank.t[:, :],
                                                           func=AF.Sigmoid), reads=[bank.b], writes=[sg.b])
                    S.dma("sp", sg_scr[d, tt * 128:(tt + 1) * 128, :], sg.t[:], reads=[sg.b])
                adt = sb(sB, "adt%d" % d, [64, T], F32)
                a2s = sb(sB, "a2s%d" % d, [64, 1024], F32)
                S.dma("sp", adt.t[:], ps_scr[3200 + 64 * d:3264 + 64 * d, :], writes=[adt.b])
                S.dma("sp", a2s.t[:], a2[d], writes=[a2s.b])
                At = [sb(sB, "at%d_%d" % (d, i), [128, T], F32) for i in range(2)]
                for cb in range(8):
                    at = At[cb % 2]
                    for tq in range(4):
                        bank = pb[nb % 4]
                        nb += 1
                        S.op("pe", lambda e: e.matmul(bank.t[:, :], lhsT=a2s.t[:, cb * 128:(cb + 1) * 128],
                                                      rhs=adt.t[:, tq * 512:(tq + 1) * 512], start=True, stop=True),
                             reads=[adt.b, a2s.b], writes=[bank.b])
                        S.op("act", lambda e: e.activation(out=at.t[:, tq * 512:(tq + 1) * 512], in_=bank.t[:, :],
                                                           func=AF.Sigmoid, bias=A0(d, cb)), reads=[bank.b], writes=[at.b])
                    S.dma("sp", a_scr[d, cb * 128:(cb + 1) * 128, :], at.t[:], reads=[at.b])
            S.barrier()
        if upto == "B":
            sG.close()
            sC.close()
            return nc

        with sG, ExitStack() as sS:
            R_ = sb(sS, "r_sb", [128, T], F32)
            K_ = sb(sS, "k_sb", [128, T], F32)
            V_ = sb(sS, "v_sb", [128, T], F32)
            KKt = sb(sS, "kk_sb", [128, T], F32)
            RKt = sb(sS, "rk_sb", [128, T], F32)
            TMP = sb(sS, "tmp_sb", [128, T], F32)
            TMP2 = sb(sS, "tmp2_sb", [128, T], F32)
            Vb = sb(sS, "v_bf", [128, T], BF16)
            A_ = sb(sS, "a_sb", [128, T], F32)
            SGS = sb(sS, "sgslab", [128, NT, 128], F32)
            EG = sb(sS, "eg", [128, T], F32)
            EGX = sb(sS, "egx", [128, T], F32)
            ENG_ = sb(sS, "eng", [128, T], F32)
            GAM = [sb(sS, "gam%d" % d, [128, NT], F32) for d in range(2)]
            OPS = [sb(sS, "ops%d" % d, [128, 4, T], BF16) for d in range(2)]
            YD = [sb(sS, "yd%d" % d, [128, T], F32) for d in range(2)]
            TMr = [sb(sS, "tm%d" % i, [128, 6, 128], BF16) for i in range(2)]
            ATr = [sb(sS, "at3_%d" % i, [128, 4, 384], BF16) for i in range(2)]
            Qr = [sb(sS, "q%d" % i, [128, 4, 128], BF16) for i in range(2)]
            LVr = [sb(sS, "lv%d" % i, [128, 4, 256], BF16) for i in range(2)]
            LVb2 = [Buf() for _ in range(2)]
            Wsb = sb(sS, "wsb", [128, 256], BF16)
            Usb = sb(sS, "usb", [128, 256], BF16)
            Hf = sb(sS, "hf", [128, 2, 64], F32)
            Hb = sb(sS, "hb", [128, 2, 64], BF16)
            MIXo = sb(sS, "mixo", [128, T], BF16)
            PXXt = es.enter_context(nc.psum_tensor("pxx_dummy", [1, 1], F32)) if False else None
            PW = PU = pb[5].b
            PY = PH = pb[6].b
            pw_ap = lambda ii: pb[5].t[:, ii * 64:(ii + 1) * 64]
            pu_ap = lambda ii: pb[5].t[:, 256 + ii * 64:256 + (ii + 1) * 64]
            py_ap = lambda d, hh: pb[6].t[64 * hh:64 * hh + 64, d * 128:(d + 1) * 128]
            ph_ap = lambda d, hh: pb[6].t[64 * hh:64 * hh + 64, 256 + d * 64:256 + (d + 1) * 64]
            sgv = [sg_scr[d].rearrange("(tt j) c -> j tt c", j=128) for d in range(2)]

            def chunk_of(d, s):
                return s if d == 0 else NT - 1 - s

            for cb in range(8):
                S.dma("sp", R_.t[:], ps_scr[cb * 128:(cb + 1) * 128, :], writes=[R_.b])
                S.dma("sp", K_.t[:], ps_scr[1024 + cb * 128:1024 + (cb + 1) * 128, :], writes=[K_.b])
                S.dma("sp", V_.t[:], ps_scr[2048 + cb * 128:2048 + (cb + 1) * 128, :], writes=[V_.b])
                S.op("dve", lambda e: e.tensor_scalar(out=KKt.t[:], in0=K_.t[:], scalar1=KK_(cb), scalar2=None, op0=ALU.mult),
                     reads=[K_.b], writes=[KKt.b])
                S.op("dve", lambda e: e.tensor_tensor(out=TMP.t[:], in0=KKt.t[:], in1=KKt.t[:], op=ALU.mult), reads=[KKt.b],
                     writes=[TMP.b])
                for tq in range(4):
                    bank = pb[tq % 4]
                    S.op("pe", lambda e: e.matmul(bank.t[:, :], lhsT=BO1, rhs=TMP.t[:, tq * 512:(tq + 1) * 512], start=True,
                                                  stop=True), reads=[TMP.b], writes=[bank.b])
                    S.op("act", lambda e: e.activation(out=TMP2.t[:, tq * 512:(tq + 1) * 512], in_=bank.t[:, :], func=AF.Sqrt),
                         reads=[bank.b], writes=[TMP2.b])
                S.op("dve", lambda e: e.tensor_scalar(out=TMP2.t[:], in0=TMP2.t[:], scalar1=1e-12, scalar2=None, op0=ALU.max),
                     reads=[TMP2.b], writes=[TMP2.b])
                S.op("dve", lambda e: e.reciprocal(out=TMP2.t[:], in_=TMP2.t[:]), reads=[TMP2.b], writes=[TMP2.b])
                S.op("dve", lambda e: e.tensor_tensor(out=KKt.t[:], in0=KKt.t[:], in1=TMP2.t[:], op=ALU.mult),
                     reads=[KKt.b, TMP2.b], writes=[KKt.b])
                S.op("act", lambda e: e.activation(out=Vb.t[:], in_=V_.t[:], func=AF.Copy), reads=[V_.b], writes=[Vb.b])
                S.op("dve", lambda e: e.scalar_tensor_tensor(out=RKt.t[:], in0=R_.t[:], scalar=RK_(cb), in1=K_.t[:],
                                                             op0=ALU.mult, op1=ALU.mult), reads=[R_.b, K_.b], writes=[RKt.b])
                for d in range(2):
                    S.dma("sp", A_.t[:], a_scr[d, cb * 128:(cb + 1) * 128, :], writes=[A_.b])
                    S.dma("sp", SGS.t[:], sgv[d][:, :, cb * 128:(cb + 1) * 128], writes=[SGS.b])
                    for tp in range(8):
                        bank = pb[tp % 4]
                        for u in range(2):
                            tt = 2 * tp + u
                            S.op("pe", lambda e: e.matmul(bank.t[:, u * 256:(u + 1) * 256], lhsT=SGS.t[:, tt, :], rhs=TRI[d],
                                                          start=True, stop=True), reads=[SGS.b], writes=[bank.b], inc=(u == 1))
                        bv = bank.t[:, :].rearrange("p (u x i) -> p u x i", u=2, x=2)
                        dst = lambda X: X.t[:, tp * 256:(tp + 1) * 256].rearrange("p (u i) -> p u i", u=2)
                        S.op("act", lambda e: e.activation(out=dst(EG), in_=bv[:, :, 0, :], func=AF.Exp), reads=[bank.b],
                             writes=[EG.b])
                        S.op("act", lambda e: e.activation(out=dst(EGX), in_=bv[:, :, 1, :], func=AF.Exp), reads=[bank.b],
                             writes=[EGX.b])
                        S.op("act", lambda e: e.activation(out=dst(ENG_), in_=bv[:, :, 0, :], func=AF.Exp, scale=-1.0),
                             reads=[bank.b], writes=[ENG_.b])
                    gi = 127 if d == 0 else 0
                    S.op("dve", lambda e: e.tensor_copy(out=GAM[d].t[:],
                                                        in_=EG.t[:].rearrange("p (tt i) -> p tt i", i=128)[:, :, gi]),
                         reads=[EG.b], writes=[GAM[d].b])
                    S.op("dve", lambda e: e.scalar_tensor_tensor(out=OPS[d].t[:, 0, :], in0=KKt.t[:], scalar=-1.0, in1=EGX.t[:],
                                                                 op0=ALU.mult, op1=ALU.mult), reads=[KKt.b, EGX.b],
                         writes=[OPS[d].b])
                    S.op("dve", lambda e: e.tensor_tensor(out=OPS[d].t[:, 1, :], in0=R_.t[:], in1=EG.t[:], op=ALU.mult),
                         reads=[R_.b, EG.b], writes=[OPS[d].b])
                    S.op("dve", lambda e: e.tensor_tensor(out=TMP.t[:], in0=KKt.t[:], in1=A_.t[:], op=ALU.mult),
                         reads=[KKt.b, A_.b], writes=[TMP.b])
                    S.op("dve", lambda e: e.tensor_tensor(out=OPS[d].t[:, 2, :], in0=TMP.t[:], in1=ENG_.t[:], op=ALU.mult),
                         reads=[TMP.b, ENG_.b], writes=[OPS[d].b])
                    S.op("dve", lambda e: e.tensor_scalar(out=TMP.t[:], in0=A_.t[:], scalar1=KA_(cb), scalar2=OMKA(cb),
                                                          op0=ALU.mult, op1=ALU.add), reads=[A_.b], writes=[TMP.b])
                    S.op("dve", lambda e: e.tensor_tensor(out=TMP.t[:], in0=TMP.t[:], in1=K_.t[:], op=ALU.mult),
                         reads=[TMP.b, K_.b], writes=[TMP.b])
                    S.op("dve", lambda e: e.tensor_tensor(out=OPS[d].t[:, 3, :], in0=TMP.t[:], in1=ENG_.t[:], op=ALU.mult),
                         reads=[TMP.b, ENG_.b], writes=[OPS[d].b])
                if debug.get("ops_dump") == cb:
                    for d in range(2):
                        for a in range(4):
                            S.op("act", lambda e: e.activation(out=TMP.t[:], in_=OPS[d].t[:, a, :], func=AF.Copy),
                                 reads=[OPS[d].b], writes=[TMP.b])
                            S.dma("sp", dbg[(d * 4 + a) * 128:(d * 4 + a + 1) * 128, :], TMP.t[:], reads=[TMP.b])
                    S.dma("sp", dbg[1024:1152, 0:16], GAM[0].t[:], reads=[GAM[0].b])
                    S.dma("sp", dbg[1152:1280, 0:16], GAM[1].t[:], reads=[GAM[1].b])

                if debug.get("prep_only"):
                    break
                S.op("dve", lambda e: e.memset(Hf.t[:], 0.0), writes=[Hf.b])
                S.op("dve", lambda e: e.memset(Hb.t[:], 0.0), writes=[Hb.b])

                def inv_gen(s):
                    par = s % 2
                    TM, AT, Q = TMr[par], ATr[par], Qr[par]
                    trs = [slice(chunk_of(d, s) * 128, chunk_of(d, s) * 128 + 128) for d in range(2)]
                    srcs = []
                    for d in range(2):
                        srcs += [(OPS[d].t[:, 2, trs[d]], OPS[d].b), (OPS[d].t[:, 3, trs[d]], OPS[d].b), (Vb.t[:, trs[d]], Vb.b)]
                    for i, (ap_, b_) in enumerate(srcs):
                        S.op("pe", lambda e: e.transpose(ptr.t[:, i * 128:(i + 1) * 128], ap_, IDB), reads=[b_],
                             writes=[ptr.b], inc=(i == 5))
                    S.op("act", lambda e: e.activation(out=TM.t[:].rearrange("p a i -> p (a i)"), in_=ptr.t[:, 0:768],
                                                       func=AF.Copy), reads=[ptr.b], writes=[TM.b])
                    yield
                    for d in range(2):
                        for hh in range(2):
                            ii = 2 * d + hh
                            rs = slice(64 * hh, 64 * hh + 64)
                            aT = OPS[d].t[rs, 0, trs[d]]
                            bT = OPS[d].t[rs, 2, trs[d]]
                            kT = OPS[d].t[rs, 3, trs[d]]
                            arT = OPS[d].t[rs, 0:2, trs[d]]
                            xb = pb[hh]
                            xo = d * 256
                            S.op("pe", lambda e: e.matmul(xb.t[:, xo:xo + 128], lhsT=bT, rhs=aT, start=True, stop=True),
                                 reads=[OPS[d].b], writes=[xb.b], inc=False)
                            S.op("pe", lambda e: e.matmul(xb.t[:, xo + 128:xo + 256], lhsT=aT, rhs=bT, start=True, stop=True),
                                 reads=[OPS[d].b], writes=[xb.b], inc=False)
                            ab = pb[2 + hh]
                            S.op("pe", lambda e: e.matmul(ab.t[:, 0:128], lhsT=bT, rhs=OPS[d].t[rs, 1, trs[d]], start=True,
                                                          stop=True), reads=[OPS[d].b], writes=[ab.b], inc=False)
                            S.op("pe", lambda e: e.matmul(ab.t[:, 128:384].rearrange("p (a i) -> p a i", a=2), lhsT=kT, rhs=arT,
                                                          start=True, stop=True), reads=[OPS[d].b], writes=[ab.b], inc=True)
                            S.op("dve", lambda e: e.tensor_tensor(out=AT.t[:, ii, :], in0=ab.t[:, 0:384], in1=M3[d][:, 0:384],
                                                                  op=ALU.mult), reads=[ab.b], writes=[AT.b])
                        yield
                    LV0 = LVr[0]
                    for h2 in range(2):
                        S.op("dve", lambda e: e.tensor_tensor(
                            out=LV0.t[:].rearrange("p (d h) x -> p d h x", h=2)[:, :, h2, :],
                            in0=pb[h2].t[:, :].rearrange("p (d x) -> p d x", d=2),
                            in1=MXX[:, 256:768].rearrange("p (d x) -> p d x", d=2),
                            op=ALU.mult), reads=[pb[h2].b], writes=[LV0.b, LVb2[0]])
                    S.op("dve", lambda e: e.tensor_tensor(out=Q.t[:], in0=LV0.t[:, :, 0:128],
                                                          in1=ID4.rearrange("p (a i) -> p a i", a=4), op=ALU.add),
                         reads=[LV0.b, LVb2[0]], writes=[Q.b])
                    yield
                    def sq(n):
                        LVp, LVn = LVr[(n - 1) % 2], LVr[n % 2]
                        for ii in range(4):
                            lb = pb[ii // 2]
                            lo = (ii % 2) * 256
                            pbuf = LVp.b if ii < 2 else LVb2[(n - 1) % 2]
                            if n < 6:
                                S.op("pe", lambda e: e.matmul(lb.t[:, lo:lo + 128], lhsT=LVp.t[:, ii, 128:256],
                                                              rhs=LVp.t[:, ii, 0:128], start=True, stop=True), reads=[pbuf],
                                     writes=[lb.b], inc=False)
                            S.op("pe", lambda e: e.matmul(lb.t[:, lo + 128:lo + 256], lhsT=LVp.t[:, ii, 0:128],
                                                          rhs=LVp.t[:, ii, 128:256], start=True, stop=True), reads=[pbuf],
                                 writes=[lb.b], inc=(ii % 2 == 1))
                        S.op("act", lambda e: e.activation(out=LVn.t[:, 0:2, :].rearrange("p a x -> p (a x)"),
                                                           in_=pb[0].t[:, :], func=AF.Copy), reads=[pb[0].b], writes=[LVn.b])
                        S.op("dve", lambda e: e.tensor_copy(out=LVn.t[:, 2:4, :].rearrange("p a x -> p (a x)"),
                                                            in_=pb[1].t[:, :]), reads=[pb[1].b], writes=[LVb2[n % 2]])

                    def qmm(n):
                        LVn = LVr[n % 2]
                        for ii in range(4):
                            S.op("pe", lambda e: e.matmul(pb[4].t[:, ii * 128:(ii + 1) * 128], lhsT=LVn.t[:, ii, 128:256],
                                                          rhs=Q.t[:, ii, :], start=True, stop=True),
                                 reads=[LVn.b if ii < 2 else LVb2[n % 2], Q.b], writes=[pb[4].b], inc=(ii == 3))
                        S.op("dve", lambda e: e.tensor_tensor(out=Q.t[:].rearrange("p a i -> p (a i)"), in0=pb[4].t[:, :],
                                                              in1=Q.t[:].rearrange("p a i -> p (a i)"), op=ALU.add),
                             reads=[pb[4].b, Q.b], writes=[Q.b])

                    sq(1)
                    yield
                    for n in range(2, 7):
                        sq(n)
                        qmm(n - 1)
                        yield
                    qmm(6)
                    yield

                def chain_gen(s):
                    par = s % 2
                    TM, AT, Q = TMr[par], ATr[par], Qr[par]
                    cds = [chunk_of(d, s) for d in range(2)]
                    trs = [slice(c_ * 128, c_ * 128 + 128) for c_ in cds]
                    for d in range(2):
                        for hh in range(2):
                            ii = 2 * d + hh
                            rs = slice(64 * hh, 64 * hh + 64)
                            S.op("pe", lambda e: e.matmul(pw_ap(ii), lhsT=OPS[d].t[rs, 0, trs[d]], rhs=Hb.t[rs, d, :], start=True,
                                                          stop=False), reads=[OPS[d].b, Hb.b], writes=[PW], inc=False)
                            S.op("pe", lambda e: e.matmul(pw_ap(ii), lhsT=AT.t[:, ii, 128:256],
                                                          rhs=TM.t[:, 3 * d + 2, hh * 64:(hh + 1) * 64], start=False, stop=True),
                                 reads=[AT.b, TM.b], writes=[PW], inc=(ii == 3))
                    S.op("act", lambda e: e.activation(out=Wsb.t[:], in_=pb[5].t[:, 0:256], func=AF.Copy), reads=[PW],
                         writes=[Wsb.b])
                    yield
                    for ii in range(4):
                        S.op("pe", lambda e: e.matmul(pu_ap(ii), lhsT=Q.t[:, ii, :], rhs=Wsb.t[:, ii * 64:(ii + 1) * 64],
                                                      start=True, stop=True), reads=[Q.b, Wsb.b], writes=[PU], inc=(ii == 3))
                    S.op("act", lambda e: e.activation(out=Usb.t[:], in_=pb[5].t[:, 256:512], func=AF.Copy), reads=[PU],
                         writes=[Usb.b])
                    yield
                    for d in range(2):
                        for hh in range(2):
                            ii = 2 * d + hh
                            rs = slice(64 * hh, 64 * hh + 64)
                            vtm = TM.t[:, 3 * d + 2, hh * 64:(hh + 1) * 64]
                            S.op("pe", lambda e: e.matmul(py_ap(d, hh), lhsT=Hb.t[rs, d, :], rhs=OPS[d].t[rs, 1, trs[d]],
                                                          start=True, stop=False), reads=[Hb.b, OPS[d].b], writes=[PY], inc=False)
                            S.op("pe", lambda e: e.matmul(py_ap(d, hh), lhsT=Usb.t[:, ii * 64:(ii + 1) * 64],
                                                          rhs=AT.t[:, ii, 0:128], start=False, stop=False), reads=[Usb.b, AT.b],
                                 writes=[PY], inc=False)
                            S.op("pe", lambda e: e.matmul(py_ap(d, hh), lhsT=vtm, rhs=AT.t[:, ii, 256:384], start=False,
                                                          stop=True), reads=[TM.b, AT.b], writes=[PY], inc=False)
                            S.op("pe", lambda e: e.matmul(ph_ap(d, hh), lhsT=TM.t[:, 3 * d, hh * 64:(hh + 1) * 64],
                                                          rhs=Usb.t[:, ii * 64:(ii + 1) * 64], start=True, stop=False),
                                 reads=[TM.b, Usb.b], writes=[PH], inc=False)
                            S.op("pe", lambda e: e.matmul(ph_ap(d, hh), lhsT=TM.t[:, 3 * d + 1, hh * 64:(hh + 1) * 64], rhs=vtm,
                                                          start=False, stop=True), reads=[TM.b], writes=[PH], inc=(hh == 1))
                        if d == 0:
                            yield
                    for d in range(2):
                        S.op("act", lambda e: e.activation(out=YD[d].t[:, trs[d]], in_=pb[6].t[:, d * 128:(d + 1) * 128],
                                                           func=AF.Copy), reads=[PY], writes=[YD[d].b])
                        S.op("dve", lambda e: e.tensor_tensor(out=Hf.t[:, d, :], in0=pb[6].t[:, 256 + d * 64:256 + (d + 1) * 64],
                                                              in1=Hf.t[:, d, :], op=ALU.add), reads=[PH, Hf.b], writes=[Hf.b])
                        S.op("dve", lambda e: e.tensor_scalar(out=Hf.t[:, d, :], in0=Hf.t[:, d, :],
                                                              scalar1=GAM[d].t[:, cds[d]:cds[d] + 1], scalar2=None, op0=ALU.mult),
                             reads=[Hf.b, GAM[d].b], writes=[Hf.b])
                    S.op("act", lambda e: e.activation(out=Hb.t[:], in_=Hf.t[:], func=AF.Copy), reads=[Hf.b], writes=[Hb.b])
                    yield

                def _sched(chain, inv, pattern="IIIIICICIICICII"):
                    gens = {"C": chain, "I": inv}
                    for ch in pattern:
                        if gens[ch] is not None:
                            try:
                                next(gens[ch])
                            except StopIteration:
                                gens[ch] = None
                    _interleave([g for g in gens.values() if g is not None])

                _interleave([inv_gen(0)])
                for s in range(NT):
                    _sched(chain_gen(s), inv_gen(s + 1) if s + 1 < NT else None)
                if debug.get("y_dump") == cb:
                    S.dma("sp", dbg[0:128, :], YD[0].t[:], reads=[YD[0].b])
                    S.dma("sp", dbg[128:256, :], YD[1].t[:], reads=[YD[1].b])

                S.op("dve", lambda e: e.tensor_tensor(out=YD[0].t[:], in0=YD[0].t[:], in1=YD[1].t[:], op=ALU.add),
                     reads=[YD[0].b, YD[1].b], writes=[YD[0].b])
                Y = YD[0]
                for tq in range(4):
                    ts_ = slice(tq * 512, (tq + 1) * 512)
                    bm, bv_, bb, bg = pb[0], pb[1], pb[2], pb[3]
                    S.op("pe", lambda e: e.matmul(bm.t[:, :], lhsT=BO64, rhs=Y.t[:, ts_], start=True, stop=True), reads=[Y.b],
                         writes=[bm.b])
                    S.op("dve", lambda e: e.tensor_tensor(out=TMP.t[:, ts_], in0=Y.t[:, ts_], in1=bm.t[:, :], op=ALU.subtract),
                         reads=[Y.b, bm.b], writes=[TMP.b])
                    S.op("act", lambda e: e.activation(out=TMP2.t[:, ts_], in_=TMP.t[:, ts_], func=AF.Square), reads=[TMP.b],
                         writes=[TMP2.b])
                    S.op("pe", lambda e: e.matmul(bv_.t[:, :], lhsT=BO64, rhs=TMP2.t[:, ts_], start=True, stop=True),
                         reads=[TMP2.b], writes=[bv_.b])
                    S.op("dve", lambda e: e.tensor_scalar(out=TMP2.t[:, ts_], in0=bv_.t[:, :], scalar1=GN_EPS, scalar2=None,
                                                          op0=ALU.add), reads=[bv_.b], writes=[TMP2.b])
                    S.op("act", lambda e: e.activation(out=TMP2.t[:, ts_], in_=TMP2.t[:, ts_], func=AF.Sqrt), reads=[TMP2.b],
                         writes=[TMP2.b])
                    S.op("dve", lambda e: e.reciprocal(out=TMP2.t[:, ts_], in_=TMP2.t[:, ts_]), reads=[TMP2.b], writes=[TMP2.b])
                    S.op("dve", lambda e: e.tensor_tensor(out=TMP.t[:, ts_], in0=TMP.t[:, ts_], in1=TMP2.t[:, ts_], op=ALU.mult),
                         reads=[TMP.b, TMP2.b], writes=[TMP.b])
                    S.op("dve", lambda e: e.tensor_scalar(out=TMP.t[:, ts_], in0=TMP.t[:, ts_], scalar1=LG_(cb), scalar2=LB_(cb),
                                                          op0=ALU.mult, op1=ALU.add), reads=[TMP.b], writes=[TMP.b])
                    S.op("pe", lambda e: e.matmul(bb.t[:, :], lhsT=BO1, rhs=RKt.t[:, ts_], start=True, stop=True), reads=[RKt.b],
                         writes=[bb.b])
                    S.op("dve", lambda e: e.tensor_tensor(out=TMP2.t[:, ts_], in0=V_.t[:, ts_], in1=bb.t[:, :], op=ALU.mult),
                         reads=[V_.b, bb.b], writes=[TMP2.b])
                    S.op("dve", lambda e: e.tensor_tensor(out=TMP.t[:, ts_], in0=TMP.t[:, ts_], in1=TMP2.t[:, ts_], op=ALU.add),
                         reads=[TMP.b, TMP2.b], writes=[TMP.b])
                    S.op("pe", lambda e: e.matmul(bg.t[:, :], lhsT=G2A.t[:, cb * 128:(cb + 1) * 128], rhs=SGD0.t[:, ts_],
                                                  start=True, stop=False), reads=[G2A.b, SGD0.b], writes=[bg.b], inc=False)
                    S.op("pe", lambda e: e.matmul(bg.t[:, :], lhsT=G2B.t[:, cb * 128:(cb + 1) * 128], rhs=SGD1.t[:, ts_],
                                                  start=False, stop=True), reads=[G2B.b, SGD1.b], writes=[bg.b])
                    S.op("dve", lambda e: e.tensor_tensor(out=MIXo.t[:, ts_], in0=TMP.t[:, ts_], in1=bg.t[:, :], op=ALU.mult),
                         reads=[TMP.b, bg.b], writes=[MIXo.b])
                S.dma("sp", mix_scr[cb * 128:(cb + 1) * 128, :], MIXo.t[:], reads=[MIXo.b])
                if debug.get("ncb") == cb + 1:
                    break
            S.barrier()
        sC.close()
        if upto == "S":
            return nc

        IDF2, IOTA2, EBASE2 = CPS.t[:, 0:128], CPS.t[:, 384:768], CPS.t[:, 768:800]
        inv_d = float(1.0 / D)

        def layer_norm(Z, Gt, Bt, st, junk):
            S.op("dve", lambda e: e.memset(st.t[:, 0:2], 0.0), writes=[st.b])
            S.op("act", lambda e: e.activation(out=junk.t[:], in_=Z.t[:], func=AF.Copy, accum_out=st.t[:, 0:1]),
                 reads=[Z.b], writes=[junk.b, st.b])
            S.op("act", lambda e: e.activation(out=junk.t[:], in_=Z.t[:], func=AF.Square, accum_out=st.t[:, 1:2]),
                 reads=[Z.b], writes=[junk.b, st.b])
            S.op("dve", lambda e: e.tensor_scalar(out=st.t[:, 2:3], in0=st.t[:, 0:1], scalar1=inv_d, scalar2=None,
                                                  op0=ALU.mult), reads=[st.b], writes=[st.b])
            S.op("dve", lambda e: e.tensor_tensor(out=st.t[:, 3:4], in0=st.t[:, 2:3], in1=st.t[:, 2:3], op=ALU.mult),
                 reads=[st.b], writes=[st.b])
            S.op("dve", lambda e: e.scalar_tensor_tensor(out=st.t[:, 4:5], in0=st.t[:, 1:2], scalar=inv_d, in1=st.t[:, 3:4],
                                                         op0=ALU.mult, op1=ALU.subtract), reads=[st.b], writes=[st.b])
            S.op("dve", lambda e: e.tensor_scalar(out=st.t[:, 5:6], in0=st.t[:, 4:5], scalar1=LN_EPS, scalar2=None,
                                                  op0=ALU.add), reads=[st.b], writes=[st.b])
            S.op("act", lambda e: e.activation(out=st.t[:, 6:7], in_=st.t[:, 5:6], func=AF.Sqrt), reads=[st.b], writes=[st.b])
            S.op("dve", lambda e: e.reciprocal(out=st.t[:, 7:8], in_=st.t[:, 6:7]), reads=[st.b], writes=[st.b])
            S.op("dve", lambda e: e.tensor_scalar(out=st.t[:, 8:9], in0=st.t[:, 2:3], scalar1=st.t[:, 7:8], scalar2=-1.0,
                                                  op0=ALU.mult, op1=ALU.mult), reads=[st.b], writes=[st.b])
            S.op("act", lambda e: e.activation(out=Z.t[:], in_=Z.t[:], func=AF.Identity, scale=st.t[:, 7:8],
                                               bias=st.t[:, 8:9]), reads=[Z.b, st.b], writes=[Z.b])
            S.op("dve", lambda e: e.tensor_tensor(out=Z.t[:], in0=Z.t[:], in1=Gt.t[:], op=ALU.mult), reads=[Z.b, Gt.b],
                 writes=[Z.b])
            S.op("dve", lambda e: e.tensor_tensor(out=Z.t[:], in0=Z.t[:], in1=Bt.t[:], op=ALU.add), reads=[Z.b, Bt.b],
                 writes=[Z.b])

        sH = ExitStack()
        BGU = sb(sH, "bgu", [128, NE * 32], F32)
        S.dma("sp", BGU.t[:], bgu_t, writes=[BGU.b])
        with ExitStack() as sO:
            WO = sb(sO, "wo", [128, 16, D], BF16)
            w_out_v = w_out.rearrange("(kc p) c -> p kc c", p=128)
            for kq in range(8):
                S.dma("pool", WO.t[:, kq * 2:(kq + 1) * 2, :], w_out_v[:, kq * 2:(kq + 1) * 2, :], writes=[WO.b])
            LNG = sb(sO, "lng", [128, D], F32)
            LNB = sb(sO, "lnb", [128, D], F32)
            S.dma("sp", LNG.t[:], rows[0:1, :].broadcast_to([128, D]), writes=[LNG.b])
            S.dma("sp", LNB.t[:], rows[1:2, :].broadcast_to([128, D]), writes=[LNB.b])
            BR = sb(sO, "br", [128, NE], F32)
            S.dma("sp", BR.t[:], rows[4:5, 0:NE].broadcast_to([128, NE]), writes=[BR.b])
            WR = sb(sO, "wr", [128, 16, NE], F32)
            S.dma("sp", WR.t[:], w_router.rearrange("(kc p) e -> p kc e", p=128), writes=[WR.b])
            MASKB = sb(sO, "maskb", [128, NT, NE], BF16)
            MTr = [sb(sO, "mt%d" % i, [128, 16, 128], BF16) for i in range(2)]
            XTr = [sb(sO, "xt%d" % i, [128, D], F32) for i in range(1)]
            Hr = [sb(sO, "h%d" % i, [128, D], F32) for i in range(2)]
            HBr = [sb(sO, "hb%d" % i, [128, D], BF16) for i in range(2)]
            HT = sb(sO, "ht", [128, 16, 128], F32)
            STr = [sb(sO, "lnst%d" % i, [128, 16], F32) for i in range(2)]
            sm = lambda n, w=NE: sb(sO, n, [128, w], F32)
            Lt, M8, MASKF, NEGM, EX, EM, DEN, GATEt, POS, DALL, OH, T1, JNK, DESTF = (
                sm("rl"), sm("rm8", 8), sm("rmask"), sm("rnegm", 1), sm("rex"), sm("rem"), sm("rden", 2), sm("rgate"),
                sm("rpos"), sm("rdall"), sm("roh"), sm("rt1"), sm("rjnk"), sm("rdestf", 4))
            mix_v = mix_scr.rearrange("(kc p) t -> p kc t", p=128)
            for tt in range(NT):
                trs_ = slice(tt * 128, (tt + 1) * 128)
                MT, XT, H, st = MTr[tt % 2], XTr[0], Hr[tt % 2], STr[tt % 2]
                S.dma("sp", MT.t[:], mix_v[:, :, trs_], writes=[MT.b])
                S.dma("sp", XT.t[:], x_tm[trs_, :], writes=[XT.b])
                for dblk in range(4):
                    bank = pb[dblk]
                    ds_ = slice(dblk * 512, (dblk + 1) * 512)
                    for kc in range(16):
                        S.op("pe", lambda e: e.matmul(bank.t[:, :], lhsT=MT.t[:, kc, :], rhs=WO.t[:, kc, ds_],
                                                      start=(kc == 0), stop=(kc == 15)), reads=[MT.b, WO.b],
                             writes=[bank.b], inc=(kc == 15))
                    S.op("dve", lambda e: e.scalar_tensor_tensor(out=H.t[:, ds_], in0=XT.t[:, ds_], scalar=ALPHA,
                                                                 in1=bank.t[:, :], op0=ALU.mult, op1=ALU.add),
                         reads=[XT.b, bank.b], writes=[H.b])
                layer_norm(H, LNG, LNB, st, HT)
                S.dma("sp", h1_scr[trs_, :], H.t[:], reads=[H.b])
                HB = HBr[tt % 2]
                S.op("act", lambda e: e.activation(out=HB.t[:], in_=H.t[:], func=AF.Copy), reads=[H.b], writes=[HB.b])
                S.dma("sp", h1b_scr[trs_, :], HB.t[:], reads=[HB.b])
                for fq in range(4):
                    tb = pb[4 + fq % 2]
                    for i in range(4):
                        fc = fq * 4 + i
                        S.op("pe", lambda e: e.transpose(tb.t[:, i * 128:(i + 1) * 128], H.t[:, fc * 128:(fc + 1) * 128], IDF2),
                             reads=[H.b], writes=[tb.b], inc=(i == 3))
                    S.op("act", lambda e: e.activation(out=HT.t[:, fq * 4:(fq + 1) * 4, :].rearrange("p a i -> p (a i)"),
                                                       in_=tb.t[:, :], func=AF.Copy), reads=[tb.b], writes=[HT.b])
                lb = pb[6]
                for fc in range(16):
                    S.op("pe", lambda e: e.matmul(lb.t[:, 0:NE], lhsT=HT.t[:, fc, :], rhs=WR.t[:, fc, :], start=(fc == 0),
                                                  stop=(fc == 15)), reads=[HT.b, WR.b], writes=[lb.b], inc=(fc == 15))
                S.op("dve", lambda e: e.tensor_tensor(out=Lt.t[:], in0=lb.t[:, 0:NE], in1=BR.t[:], op=ALU.add),
                     reads=[lb.b, BR.b], writes=[Lt.b])
                S.op("dve", lambda e: e.max(out=M8.t[:], in_=Lt.t[:]), reads=[Lt.b], writes=[M8.b])
                S.op("dve", lambda e: e.tensor_scalar(out=MASKF.t[:], in0=Lt.t[:], scalar1=M8.t[:, 3:4], scalar2=None,
                                                      op0=ALU.is_ge), reads=[Lt.b, M8.b], writes=[MASKF.b])
                S.op("dve", lambda e: e.tensor_copy(out=MASKB.t[:, tt, :], in_=MASKF.t[:]), reads=[MASKF.b], writes=[MASKB.b])
                S.op("dve", lambda e: e.tensor_scalar(out=NEGM.t[:], in0=M8.t[:, 0:1], scalar1=-1.0, scalar2=None,
                                                      op0=ALU.mult), reads=[M8.b], writes=[NEGM.b])
                S.op("act", lambda e: e.activation(out=EX.t[:], in_=Lt.t[:], func=AF.Exp, bias=NEGM.t[:, 0:1]),
                     reads=[Lt.b, NEGM.b], writes=[EX.b])
                S.op("dve", lambda e: e.tensor_tensor(out=EM.t[:], in0=EX.t[:], in1=MASKF.t[:], op=ALU.mult),
                     reads=[EX.b, MASKF.b], writes=[EM.b])
                S.op("dve", lambda e: e.memset(DEN.t[:], 0.0), writes=[DEN.b])
                S.op("act", lambda e: e.activation(out=JNK.t[:], in_=EM.t[:], func=AF.Copy, accum_out=DEN.t[:, 0:1]),
                     reads=[EM.b], writes=[JNK.b, DEN.b])
                S.op("dve", lambda e: e.reciprocal(out=DEN.t[:, 1:2], in_=DEN.t[:, 0:1]), reads=[DEN.b], writes=[DEN.b])
                S.op("dve", lambda e: e.tensor_scalar(out=GATEt.t[:], in0=EM.t[:], scalar1=DEN.t[:, 1:2], scalar2=None,
                                                      op0=ALU.mult), reads=[EM.b, DEN.b], writes=[GATEt.b])
                S.op("pe", lambda e: e.matmul(lb.t[:, NE:2 * NE], lhsT=TRISB, rhs=MASKB.t[:, tt, :], start=True,
                                              stop=(tt == 0)), reads=[MASKB.b], writes=[lb.b], inc=(tt == 0))
                for t2 in range(tt):
                    S.op("pe", lambda e: e.matmul(lb.t[:, NE:2 * NE], lhsT=ONESB, rhs=MASKB.t[:, t2, :], start=False,
                                                  stop=(t2 == tt - 1)), reads=[MASKB.b], writes=[lb.b], inc=(t2 == tt - 1))
                S.op("dve", lambda e: e.tensor_copy(out=POS.t[:], in_=lb.t[:, NE:2 * NE]), reads=[lb.b], writes=[POS.b])
                S.op("dve", lambda e: e.tensor_tensor(out=DALL.t[:], in0=POS.t[:], in1=EBASE2, op=ALU.add), reads=[POS.b],
                     writes=[DALL.b])
                S.op("dve", lambda e: e.scalar_tensor_tensor(out=T1.t[:], in0=POS.t[:], scalar=1.0, in1=MASKF.t[:],
                                                             op0=ALU.add, op1=ALU.mult), reads=[POS.b, MASKF.b],
                     writes=[T1.b])
                S.op("dve", lambda e: e.tensor_scalar(out=POSM.t[:, tt, :], in0=T1.t[:], scalar1=-1.0, scalar2=None,
                                                      op0=ALU.add), reads=[T1.b], writes=[POSM.b])
                S.op("dve", lambda e: e.memset(DESTF.t[:], 0.0), writes=[DESTF.b])
                S.op("dve", lambda e: e.memset(GK.t[:, tt * 4:(tt + 1) * 4], 0.0), writes=[GK.b])
                for k in range(4):
                    S.op("dve", lambda e: e.tensor_scalar(out=OH.t[:], in0=Lt.t[:], scalar1=M8.t[:, k:k + 1], scalar2=None,
                                                          op0=ALU.is_equal), reads=[Lt.b, M8.b], writes=[OH.b])
                    S.op("dve", lambda e: e.tensor_tensor(out=T1.t[:], in0=OH.t[:], in1=DALL.t[:], op=ALU.mult),
                         reads=[OH.b, DALL.b], writes=[T1.b])
                    S.op("act", lambda e: e.activation(out=JNK.t[:], in_=T1.t[:], func=AF.Copy, accum_out=DESTF.t[:, k:k + 1]),
                         reads=[T1.b], writes=[JNK.b, DESTF.b])
                    S.op("dve", lambda e: e.tensor_tensor(out=T1.t[:], in0=OH.t[:], in1=GATEt.t[:], op=ALU.mult),
                         reads=[OH.b, GATEt.b], writes=[T1.b])
                    S.op("act", lambda e: e.activation(out=JNK.t[:], in_=T1.t[:], func=AF.Copy,
                                                       accum_out=GK.t[:, tt * 4 + k:tt * 4 + k + 1]),
                         reads=[T1.b], writes=[JNK.b, GK.b])
                S.op("dve", lambda e: e.tensor_copy(out=DEST.t[:, tt * 4:(tt + 1) * 4], in_=DESTF.t[:]), reads=[DESTF.b],
                     writes=[DEST.b])
                if "dbg" in debug.get("dump", ()):
                    S.op("dve", lambda e: e.tensor_copy(out=OH.t[:, 0:4], in_=DEST.t[:, tt * 4:(tt + 1) * 4]), reads=[DEST.b],
                         writes=[OH.b])
                    S.dma("sp", dbg[trs_, 136:140], OH.t[:, 0:4], reads=[OH.b])
                    for j, X_ in enumerate((Lt, MASKF, GATEt, POS)):
                        S.dma("sp", dbg[trs_, j * NE:(j + 1) * NE], X_.t[:], reads=[X_.b])
                    S.dma("sp", dbg[trs_, 128:132], DESTF.t[:], reads=[DESTF.b])
                    S.dma("sp", dbg[trs_, 132:136], GK.t[:, tt * 4:(tt + 1) * 4], reads=[GK.b])
            S.barrier()
        if upto == "O":
            sH.close()
            return nc

        NEX = debug.get("nex", NE)
        with sH, ExitStack() as sE:
            TIDB = sb(sE, "tidb", [128, NT, 2], BF16)
            TIDF = sb(sE, "tidf", [128, 2 * NT], F32)
            S.dma("sp", TIDF.t[:], tid, writes=[TIDF.b])
            S.op("dve", lambda e: e.tensor_copy(out=TIDB.t[:].rearrange("p a b -> p (a b)"), in_=TIDF.t[:]), reads=[TIDF.b],
                 writes=[TIDB.b])
            PEr = [sb(sE, "pe%d" % i, [128, NT, CAP], BF16) for i in range(1)]
            IDXF = [sb(sE, "idxf%d" % i, [128, NJ], F32) for i in range(2)]
            IDXR = [sb(sE, "idxr%d" % i, [128, 2 * NJ], F32) for i in range(2)]
            IDXs = [sb(sE, "idx%d" % i, [128, NJ], I32) for i in range(NEX)]
            XGr = [[sb(sE, "xg%d_%d" % (i, j), [128, D], BF16) for j in range(NJ)] for i in range(2)]
            XeTr = [sb(sE, "xet%d" % i, [128, 16, CAP], BF16) for i in range(2)]
            NWB = 12
            WBr = [sb(sE, "wb%d" % i, [128, 16, 256], BF16) for i in range(NWB)]
            ACTT = sb(sE, "actt", [128, 16, CAP], BF16)
            G32r = [sb(sE, "g32_%d" % i, [128, CAP], F32) for i in range(2)]
            S32r = [sb(sE, "s32_%d" % i, [128, CAP], F32) for i in range(2)]
            L32r = [sb(sE, "l32_%d" % i, [128, CAP], F32) for i in range(2)]
            BD = sb(sE, "bd", [128, D], F32)
            Yer = [sb(sE, "ye%d" % i, [128, 256], F32) for i in range(4)]
            cnt = {"bk": 0, "ye": 0, "pt": 0, "wb": 0}

            def next_bank():
                b_ = pb[cnt["bk"] % 6]
                cnt["bk"] += 1
                return b_

            def next_wb():
                w_ = WBr[cnt["wb"] % NWB]
                cnt["wb"] += 1
                return w_

            def build_idx(ex):
                PEe = PEr[0]
                for tt in range(NT):
                    S.op("dve", lambda e: e.tensor_scalar(out=PEe.t[:, tt, :], in0=IOTA2, scalar1=POSM.t[:, tt, ex:ex + 1],
                                                          scalar2=None, op0=ALU.is_equal), reads=[POSM.b], writes=[PEe.b])
                ib = pb[6]
                for jc in range(NJ):
                    for tt in range(NT):
                        S.op("pe", lambda e: e.matmul(ib.t[:, jc * 2:(jc + 1) * 2], lhsT=PEe.t[:, tt, jc * 128:(jc + 1) * 128],
                                                      rhs=TIDB.t[:, tt, :], start=(tt == 0), stop=(tt == NT - 1)),
                             reads=[PEe.b, TIDB.b], writes=[ib.b], inc=(tt == NT - 1))
                xf = IDXF[ex % 2]
                xr = IDXR[ex % 2]
                S.op("dve", lambda e: e.tensor_copy(out=xr.t[:], in_=ib.t[:, 0:2 * NJ]), reads=[ib.b], writes=[xr.b])
                iv = xr.t[:].rearrange("p (j c) -> p j c", c=2)
                S.op("dve", lambda e: e.scalar_tensor_tensor(out=xf.t[:], in0=iv[:, :, 1], scalar=128.0, in1=iv[:, :, 0],
                                                             op0=ALU.mult, op1=ALU.add), reads=[xr.b], writes=[xf.b])
                S.op("dve", lambda e: e.tensor_copy(out=IDXs[ex].t[:], in_=xf.t[:]), reads=[xf.b], writes=[IDXs[ex].b])

            def gather(ex):
                for jc in range(NJ):
                    xg = XGr[ex % 2][jc]
                    S.gather(xg.t[:], h1b_scr, IDXs[ex].t[:, jc:jc + 1], reads=[IDXs[ex].b], writes=[xg.b])

            def tr_round(ex, r):
                XeT = XeTr[ex % 2]
                for u in range(2):
                    fc = 2 * r + u
                    for jc in range(NJ):
                        xg = XGr[ex % 2][jc]
                        S.op("pe", lambda e: e.transpose(ptr.t[:, u * CAP + jc * 128:u * CAP + (jc + 1) * 128],
                                                         xg.t[:, fc * 128:(fc + 1) * 128], IDB), reads=[xg.b], writes=[ptr.b],
                             inc=(u == 1 and jc == NJ - 1))
                S.op("act", lambda e: e.activation(out=XeT.t[:, 2 * r:2 * r + 2, :].rearrange("p a j -> p (a j)"),
                                                   in_=ptr.t[:, 0:2 * CAP], func=AF.Copy), reads=[ptr.b], writes=[XeT.b])

            build_idx(0)
            if NEX > 1:
                build_idx(1)
            gather(0)
            for r in range(8):
                tr_round(0, r)
            for ex in range(NEX):
                XeT = XeTr[ex % 2]
                if ex + 2 < NEX:
                    build_idx(ex + 2)
                if ex + 1 < NEX:
                    gather(ex + 1)
                S.dma("sp", BD.t[:], b_down[ex:ex + 1, :].broadcast_to([128, D]), writes=[BD.b])
                wgv = w_gu[ex].rearrange("(kc p) c -> p kc c", p=128)
                for pblk in range(8):
                    WG, WL = next_wb(), next_wb()
                    S.dma("pool", WG.t[:], wgv[:, :, pblk * 256:(pblk + 1) * 256], writes=[WG.b])
                    S.dma("pool", WL.t[:], wgv[:, :, D + pblk * 256:D + (pblk + 1) * 256], writes=[WL.b])
                    for sub in range(2):
                        pt = pblk * 2 + sub
                        bg_, bl_ = next_bank(), next_bank()
                        g32, s32, l32 = G32r[cnt["pt"] % 2], S32r[cnt["pt"] % 2], L32r[cnt["pt"] % 2]
                        cnt["pt"] += 1
                        for kc in range(16):
                            S.op("pe", lambda e: e.matmul(bg_.t[:, 0:CAP], lhsT=WG.t[:, kc, sub * 128:(sub + 1) * 128],
                                                          rhs=XeT.t[:, kc, :], start=(kc == 0), stop=(kc == 15)),
                                 reads=[WG.b, XeT.b], writes=[bg_.b], inc=(kc == 15))
                        for kc in range(16):
                            S.op("pe", lambda e: e.matmul(bl_.t[:, 0:CAP], lhsT=WL.t[:, kc, sub * 128:(sub + 1) * 128],
                                                          rhs=XeT.t[:, kc, :], start=(kc == 0), stop=(kc == 15)),
                                 reads=[WL.b, XeT.b], writes=[bl_.b], inc=(kc == 15))
                        cg = ex * 32 + pt
                        cl = ex * 32 + 16 + pt
                        S.op("dve", lambda e: e.tensor_scalar(out=g32.t[:], in0=bg_.t[:, 0:CAP], scalar1=BGU.t[:, cg:cg + 1],
                                                              scalar2=7.0, op0=ALU.add, op1=ALU.min), reads=[bg_.b, BGU.b],
                             writes=[g32.b])
                        S.op("act", lambda e: e.activation(out=s32.t[:], in_=g32.t[:], func=AF.Sigmoid, scale=1.702),
                             reads=[g32.b], writes=[s32.b])
                        S.op("dve", lambda e: e.tensor_scalar(out=l32.t[:], in0=bl_.t[:, 0:CAP], scalar1=BGU.t[:, cl:cl + 1],
                                                              scalar2=7.0, op0=ALU.add, op1=ALU.min), reads=[bl_.b, BGU.b],
                             writes=[l32.b])
                        S.op("dve", lambda e: e.tensor_scalar(out=l32.t[:], in0=l32.t[:], scalar1=-7.0, scalar2=1.0,
                                                              op0=ALU.max, op1=ALU.add), reads=[l32.b], writes=[l32.b])
                        S.op("dve", lambda e: e.tensor_tensor(out=g32.t[:], in0=g32.t[:], in1=s32.t[:], op=ALU.mult),
                             reads=[g32.b, s32.b], writes=[g32.b])
                        S.op("dve", lambda e: e.tensor_tensor(out=ACTT.t[:, pt, :], in0=g32.t[:], in1=l32.t[:], op=ALU.mult),
                             reads=[g32.b, l32.b], writes=[ACTT.b])
                        if ex + 1 < NEX and 4 <= pt < 12:
                            tr_round(ex + 1, pt - 4)
                wdv = w_down[ex].rearrange("(kc p) c -> p kc c", p=128)
                for dblk in range(8):
                    WD = next_wb()
                    dsl = slice(dblk * 256, (dblk + 1) * 256)
                    S.dma("pool", WD.t[:], wdv[:, :, dsl], writes=[WD.b])
                    for jc in range(NJ):
                        bank = next_bank()
                        ye = Yer[cnt["ye"] % 4]
                        cnt["ye"] += 1
                        for fc in range(16):
                            S.op("pe", lambda e: e.matmul(bank.t[:, 0:256], lhsT=ACTT.t[:, fc, jc * 128:(jc + 1) * 128],
                                                          rhs=WD.t[:, fc, :], start=(fc == 0), stop=(fc == 15)),
                                 reads=[ACTT.b, WD.b], writes=[bank.b], inc=(fc == 15))
                        S.op("dve", lambda e: e.tensor_tensor(out=ye.t[:], in0=bank.t[:, 0:256], in1=BD.t[:, dsl], op=ALU.add),
                             reads=[bank.b, BD.b], writes=[ye.b])
                        S.dma("sp", y_scr[ex * CAP + jc * 128:ex * CAP + (jc + 1) * 128, dsl], ye.t[:], reads=[ye.b])
            S.barrier()
        if upto == "E":
            return nc

        with ExitStack() as sM:
            LNG2 = sb(sM, "lng2", [128, D], F32)
            LNB2 = sb(sM, "lnb2", [128, D], F32)
            S.dma("sp", LNG2.t[:], rows[2:3, :].broadcast_to([128, D]), writes=[LNG2.b])
            S.dma("sp", LNB2.t[:], rows[3:4, :].broadcast_to([128, D]), writes=[LNB2.b])
            H1r = [sb(sM, "h1_%d" % i, [128, D], F32) for i in range(2)]
            Ykr = [sb(sM, "yk%d" % i, [128, D], F32) for i in range(8)]
            ACr = [sb(sM, "acc%d" % i, [128, D], F32) for i in range(2)]
            JK2 = sb(sM, "jk2", [128, D], F32)
            ST2 = [sb(sM, "ln2st%d" % i, [128, 16], F32) for i in range(2)]
            for tt in range(NT):
                trs_ = slice(tt * 128, (tt + 1) * 128)
                H1, AC, st = H1r[tt % 2], ACr[tt % 2], ST2[tt % 2]
                S.dma("sp", H1.t[:], h1_scr[trs_, :], writes=[H1.b])
                yks = [Ykr[(tt % 2) * 4 + k] for k in range(4)]
                for k in range(4):
                    c = tt * 4 + k
                    S.gather(yks[k].t[:], y_scr, DEST.t[:, c:c + 1], reads=[DEST.b], writes=[yks[k].b])
                S.op("act", lambda e: e.activation(out=AC.t[:], in_=H1.t[:], func=AF.Identity, scale=ALPHA), reads=[H1.b],
                     writes=[AC.b])
                for k in range(4):
                    c = tt * 4 + k
                    S.op("dve", lambda e: e.scalar_tensor_tensor(out=AC.t[:], in0=yks[k].t[:], scalar=GK.t[:, c:c + 1],
                                                                 in1=AC.t[:], op0=ALU.mult, op1=ALU.add),
                         reads=[yks[k].b, GK.b, AC.b], writes=[AC.b])
                layer_norm(AC, LNG2, LNB2, st, JK2)
                S.dma("sp", out[trs_, :], AC.t[:], reads=[AC.b])
            S.barrier()
    return nc


def _shared_maps(inp):
    f = lambda a: np.ascontiguousarray(np.asarray(a, dtype=np.float32))
    col = lambda v: f(np.asarray(v).reshape(-1, 128).T)
    mu = np.zeros(28 * 128, np.float32)
    mu[:D_SHIFT] = np.asarray(inp["mu_shift"])[0]
    vecs = np.concatenate([col(inp["a0"][0]), col(inp["k_k"][0]), col(inp["k_a"][0]), col(inp["r_k"][0]),
                           col(inp["lnx_g"][0]), col(inp["lnx_b"][0]), col(inp["beta_f"][0]),
                           np.zeros((128, 8), np.float32)], 1)
    rows = np.zeros((6, D), np.float32)
    rows[0], rows[1] = inp["ln1_g"][0], inp["ln1_b"][0]
    rows[2], rows[3] = inp["ln2_g"][0], inp["ln2_b"][0]
    rows[4, :NE] = inp["b_router"][0]
    w2e = np.concatenate([np.asarray(inp["w2"][0]), np.asarray(inp["w0"][0])[:, None, :]], 1)
    tt = (np.arange(T)[:, None] * np.arange(T)[None, :]) % T
    ang = (2.0 * np.pi / T) * tt.astype(np.float64)
    m = {
        "w_in": f(inp["w_in"][0]), "mu_t": col(mu), "w2e": f(w2e), "a2": f(inp["a2"][0]), "g2": f(inp["g2"][0]),
        "vecs": f(vecs), "w_out": f(inp["w_out"][0]), "rows": rows, "w_router": f(inp["w_router"][0]),
        "w_gu": f(inp["w_gu"][0]),
        "bgu_t": f(np.asarray(inp["b_gu"][0]).reshape(NE, 32, 128).transpose(2, 0, 1).reshape(128, NE * 32)),
        "w_down": f(inp["w_down"][0]), "b_down": f(inp["b_down"][0]),
        "ctab": np.cos(ang).astype(np.float32), "stab": (-np.sin(ang)).astype(np.float32), "cst": _consts(),
        "tid": np.ascontiguousarray(np.stack([np.broadcast_to(np.arange(128, dtype=np.float32)[:, None], (128, NT)),
                                              np.broadcast_to(np.arange(NT, dtype=np.float32)[None, :], (128, NT))],
                                             2).reshape(128, 2 * NT)),
    }
    return m


def _core_maps(inp, shared, b):
    xb = np.asarray(inp["x"][b], dtype=np.float32)
    m = dict(shared)
    m["xT"] = np.ascontiguousarray(xb.T)
    m["x_tm"] = np.ascontiguousarray(xb)
    return m


def kernel(**inputs):
    nc = build()
    shared = _shared_maps(inputs)
    in_maps = [_core_maps(inputs, shared, b) for b in range(8)]
    res = run_bass_kernel_spmd(nc, in_maps, core_ids=list(range(8)))
    return np.stack([np.asarray(r["out"], dtype=np.float32) for r in res.results], 0)
```
